# Optimizing a Trainium2 kernel written in Bass

```python
import math
import jax, jax.numpy as jnp
from jax import lax
import numpy as np

D_MODEL = 1024
BATCH = 16
SEQ = 2048
DEPTH = 1

MEM_LEN = 256
EPS = 1e-6
M_HEADS = 4
M_HEAD_DIM = 256
M_WIDTH = M_HEADS * M_HEAD_DIM
M_CONV = 4
M_CHUNK = 128
F_BIAS_LO = 3.0
F_BIAS_HI = 6.0
A_Q_HEADS = 16
A_KV_HEADS = 2
A_GROUP = A_Q_HEADS // A_KV_HEADS
A_HEAD_DIM = 64
A_WIDTH = A_Q_HEADS * A_HEAD_DIM
A_KV_WIDTH = A_KV_HEADS * A_HEAD_DIM
WINDOW = 128
ROPE_THETA = 10000.0
MIX_WIDTH = M_WIDTH + A_WIDTH
IN_SIZES = (M_WIDTH, M_WIDTH, M_WIDTH, M_WIDTH, M_HEADS, M_HEADS, A_WIDTH, A_KV_WIDTH, A_KV_WIDTH)
IN_WIDTH = 4 * M_WIDTH + 2 * M_HEADS + A_WIDTH + 2 * A_KV_WIDTH
X_HEADS = 4
X_HEAD_DIM = D_MODEL // X_HEADS
D_FF = -(-8 * D_MODEL // (3 * 256)) * 256

kernel_name = "hybrid_mlstm_swa_sink_xattn_layer"


def _split(t, sizes):
    idx = []
    acc = 0
    for s in sizes[:-1]:
        acc += s
        idx.append(acc)
    return jnp.split(t, idx, axis=-1)


def rmsnorm(x, g):
    xf = x.astype(jnp.float32)
    r = xf * lax.rsqrt(jnp.mean(xf * xf, axis=-1, keepdims=True) + EPS)
    return (r * g.astype(jnp.float32)).astype(x.dtype)


def causal_depthwise_conv(x, w):
    K, C = w.shape
    return lax.conv_general_dilated(
        x, w[:, None, :].astype(x.dtype), window_strides=(1,), padding=[(K - 1, 0)],
        dimension_numbers=("NWC", "WIO", "NWC"), feature_group_count=C)


def rope_tables(positions):
    inv = ROPE_THETA ** (-jnp.arange(0, A_HEAD_DIM, 2, dtype=jnp.float32) / A_HEAD_DIM)
    ang = positions.astype(jnp.float32)[..., None] * inv
    return jnp.cos(ang)[:, :, None, :], jnp.sin(ang)[:, :, None, :]


def apply_rope(t, cos, sin):
    tf = t.astype(jnp.float32)
    t1, t2 = jnp.split(tf, 2, axis=-1)
    return jnp.concatenate([t1 * cos - t2 * sin, t2 * cos + t1 * sin], axis=-1).astype(t.dtype)


def mlstm_chunkwise(q, k, v, logf, ig):
    B, S, H, D = q.shape
    nc = S // M_CHUNK

    def chunks(t):
        t = t.reshape((B, nc, M_CHUNK, H) + t.shape[3:])
        return jnp.moveaxis(t, (1, 3), (0, 2))

    causal = jnp.tril(jnp.ones((M_CHUNK, M_CHUNK), dtype=bool))

    def step(carry, xs):
        C, n, m = carry
        qc, kc, vc, lf, ic = xs
        b = jnp.cumsum(lf, axis=-1)
        g = b + m[..., None]
        Dm = b[..., :, None] - b[..., None, :] + ic[..., None, :]
        Dm = jnp.where(causal, Dm, -jnp.inf)
        mj = jnp.maximum(g, jnp.max(Dm, axis=-1))
        s = jnp.einsum("bhjd,bhid->bhji", qc, kc) * jnp.exp(Dm - mj[..., None])
        wg = jnp.exp(g - mj)
        num = wg[..., None] * jnp.einsum("bhjd,bhde->bhje", qc, C) + jnp.einsum("bhji,bhie->bhje", s, vc)
        den = wg * jnp.einsum("bhjd,bhd->bhj", qc, n) + jnp.sum(s, axis=-1)
        h = num / jnp.maximum(jnp.abs(den), jnp.exp(-mj))[..., None]
        bL = b[..., -1]
        w = bL[..., None] - b + ic
        m_new = jnp.maximum(bL + m, jnp.max(w, axis=-1))
        decay = jnp.exp(bL + m - m_new)
        wi = jnp.exp(w - m_new[..., None])
        C_new = decay[..., None, None] * C + jnp.einsum("bhi,bhid,bhie->bhde", wi, kc, vc)
        n_new = decay[..., None] * n + jnp.einsum("bhi,bhid->bhd", wi, kc)
        return (C_new, n_new, m_new), h

    init = (jnp.zeros((B, H, D, D), jnp.float32), jnp.zeros((B, H, D), jnp.float32),
            jnp.zeros((B, H), jnp.float32))
    _, h = lax.scan(step, init, (chunks(q), chunks(k), chunks(v), chunks(logf), chunks(ig)))
    return jnp.transpose(h, (1, 0, 3, 2, 4)).reshape(B, S, H, D)


def sliding_window_attention(q, k, v, sinks):
    B, S = q.shape[0], q.shape[1]
    nb = S // WINDOW
    qb = q.reshape(B, nb, WINDOW, A_KV_HEADS, A_GROUP, A_HEAD_DIM)
    kb = k.reshape(B, nb, WINDOW, A_KV_HEADS, A_HEAD_DIM)
    vb = v.reshape(B, nb, WINDOW, A_KV_HEADS, A_HEAD_DIM)

    def with_prev(t):
        prev = jnp.pad(t, ((0, 0), (1, 0), (0, 0), (0, 0), (0, 0)))[:, :-1]
        return jnp.concatenate([prev, t], axis=2)

    kw, vw = with_prev(kb), with_prev(vb)
    qpos = jnp.arange(WINDOW)[:, None]
    kpos = jnp.arange(2 * WINDOW)[None, :]
    diff = WINDOW + qpos - kpos
    band = (diff >= 0) & (diff < WINDOW)
    sink = sinks.astype(jnp.float32).reshape(1, A_KV_HEADS, A_GROUP, 1, 1)
    scale = A_HEAD_DIM ** -0.5

    def block(args):
        qi, ki, vi, bi = args
        s = jnp.einsum("bqhgd,bkhd->bhgqk", qi.astype(jnp.float32), ki.astype(jnp.float32)) * scale
        valid = band & ((bi > 0) | (kpos >= WINDOW))
        s = jnp.where(valid, s, -jnp.inf)
        mx = jnp.maximum(jnp.max(s, axis=-1, keepdims=True), sink)
        p = jnp.exp(s - mx)
        denom = jnp.sum(p, axis=-1, keepdims=True) + jnp.exp(sink - mx)
        o = jnp.einsum("bhgqk,bkhd->bqhgd", p / denom, vi.astype(jnp.float32))
        return o.astype(qi.dtype)

    out = lax.map(block, (jnp.moveaxis(qb, 1, 0), jnp.moveaxis(kw, 1, 0), jnp.moveaxis(vw, 1, 0),
                          jnp.arange(nb)))
    return jnp.moveaxis(out, 0, 1).reshape(B, S, A_WIDTH)


def token_mixer(u, positions, w_in, conv_qk, f_bias, i_bias, mlstm_norm_g, attn_sinks, w_out):
    B, S, _ = u.shape
    proj = u @ w_in
    qm, km, vm, om, ipre, fpre, qa, ka, va = _split(proj, IN_SIZES)

    qk = jax.nn.silu(causal_depthwise_conv(jnp.concatenate([qm, km], axis=-1), conv_qk))
    qm, km = jnp.split(qk, 2, axis=-1)
    qh = qm.astype(jnp.float32).reshape(B, S, M_HEADS, M_HEAD_DIM)
    kh = km.astype(jnp.float32).reshape(B, S, M_HEADS, M_HEAD_DIM) * (M_HEAD_DIM ** -0.5)
    vh = vm.astype(jnp.float32).reshape(B, S, M_HEADS, M_HEAD_DIM)
    logf = jax.nn.log_sigmoid(fpre.astype(jnp.float32) + f_bias.astype(jnp.float32))
    ig = ipre.astype(jnp.float32) + i_bias.astype(jnp.float32)
    hm = mlstm_chunkwise(qh, kh, vh, logf, ig)
    hm = hm * lax.rsqrt(jnp.mean(hm * hm, axis=-1, keepdims=True) + EPS)
    hm = hm * mlstm_norm_g.astype(jnp.float32).reshape(M_HEADS, M_HEAD_DIM)
    hm = (jax.nn.sigmoid(om.astype(jnp.float32)) * hm.reshape(B, S, M_WIDTH)).astype(u.dtype)

    cos, sin = rope_tables(positions)
    qa = apply_rope(qa.reshape(B, S, A_Q_HEADS, A_HEAD_DIM), cos, sin)
    ka = apply_rope(ka.reshape(B, S, A_KV_HEADS, A_HEAD_DIM), cos, sin)
    va = va.reshape(B, S, A_KV_HEADS, A_HEAD_DIM)
    ha = sliding_window_attention(qa, ka, va, attn_sinks)

    return jnp.concatenate([hm, ha], axis=-1) @ w_out


def memory_cross_attention(u, mem_n, w_xq, w_xkv, w_xo):
    B, S, _ = u.shape
    q = (u @ w_xq).reshape(B, S, X_HEADS, X_HEAD_DIM).astype(jnp.float32)
    k, v = jnp.split(mem_n @ w_xkv, 2, axis=-1)
    k = k.reshape(B, MEM_LEN, X_HEADS, X_HEAD_DIM).astype(jnp.float32)
    v = v.reshape(B, MEM_LEN, X_HEADS, X_HEAD_DIM).astype(jnp.float32)
    p = jax.nn.softmax(jnp.einsum("bshd,bmhd->bhsm", q, k) * (X_HEAD_DIM ** -0.5), axis=-1)
    o = jnp.einsum("bhsm,bmhd->bshd", p, v).reshape(B, S, D_MODEL).astype(u.dtype)
    return o @ w_xo


def swiglu_ffn(u, w_gate_up, w_down):
    g, up = jnp.split(u @ w_gate_up, 2, axis=-1)
    return (jax.nn.silu(g) * up) @ w_down


def setup_inputs(seed: int = 0) -> dict:
    key = jax.random.key(seed)
    ks = jax.random.split(key, 24)
    f32 = jnp.float32

    def nrm(k, shape, scale):
        return jax.random.normal(k, shape, f32) * scale

    def gain(k):
        return 1.0 + 0.02 * jax.random.normal(k, (DEPTH, D_MODEL), f32)

    x = jax.random.normal(ks[0], (BATCH, SEQ, D_MODEL), f32)
    mem = jax.random.normal(ks[1], (BATCH, MEM_LEN, D_MODEL), f32)
    start = jax.random.randint(ks[2], (BATCH, 1), 0, 4096, dtype=jnp.int32)
    positions = start + jnp.arange(SEQ, dtype=jnp.int32)[None, :]
    f_bias = jnp.linspace(F_BIAS_LO, F_BIAS_HI, M_HEADS, dtype=f32)[None, :] + nrm(ks[8], (DEPTH, M_HEADS), 0.1)
    return {
        "x": x,
        "mem": mem,
        "positions": positions,
        "mix_pre_g": gain(ks[3]),
        "mix_post_g": gain(ks[4]),
        "w_in": nrm(ks[5], (DEPTH, D_MODEL, IN_WIDTH), D_MODEL ** -0.5),
        "conv_qk": nrm(ks[6], (DEPTH, M_CONV, 2 * M_WIDTH), M_CONV ** -0.5),
        "f_bias": f_bias,
        "i_bias": nrm(ks[9], (DEPTH, M_HEADS), 0.1),
        "mlstm_norm_g": 1.0 + 0.02 * jax.random.normal(ks[10], (DEPTH, M_WIDTH), f32),
        "attn_sinks": nrm(ks[11], (DEPTH, A_Q_HEADS), 0.5),
        "w_out": nrm(ks[12], (DEPTH, MIX_WIDTH, D_MODEL), MIX_WIDTH ** -0.5),
        "xattn_pre_g": gain(ks[13]),
        "xattn_post_g": gain(ks[14]),
        "mem_norm_g": gain(ks[15]),
        "w_xq": nrm(ks[16], (DEPTH, D_MODEL, D_MODEL), D_MODEL ** -0.5),
        "w_xkv": nrm(ks[17], (DEPTH, D_MODEL, 2 * D_MODEL), D_MODEL ** -0.5),
        "w_xo": nrm(ks[18], (DEPTH, D_MODEL, D_MODEL), D_MODEL ** -0.5),
        "ffn_pre_g": gain(ks[19]),
        "ffn_post_g": gain(ks[20]),
        "w_gate_up": nrm(ks[21], (DEPTH, D_MODEL, 2 * D_FF), D_MODEL ** -0.5),
        "w_down": nrm(ks[22], (DEPTH, D_FF, D_MODEL), D_FF ** -0.5),
    }


def reference(x, mem, positions, mix_pre_g, mix_post_g, w_in, conv_qk, f_bias, i_bias, mlstm_norm_g,
              attn_sinks, w_out, xattn_pre_g, xattn_post_g, mem_norm_g, w_xq, w_xkv, w_xo,
              ffn_pre_g, ffn_post_g, w_gate_up, w_down):
    h = x
    for l in range(DEPTH):
        a = token_mixer(rmsnorm(h, mix_pre_g[l]), positions, w_in[l], conv_qk[l], f_bias[l], i_bias[l],
                        mlstm_norm_g[l], attn_sinks[l], w_out[l])
        h = h + rmsnorm(a, mix_post_g[l])
        c = memory_cross_attention(rmsnorm(h, xattn_pre_g[l]), rmsnorm(mem, mem_norm_g[l]),
                                   w_xq[l], w_xkv[l], w_xo[l])
        h = h + rmsnorm(c, xattn_post_g[l])
        f = swiglu_ffn(rmsnorm(h, ffn_pre_g[l]), w_gate_up[l], w_down[l])
        h = h + rmsnorm(f, ffn_post_g[l])
    return h
```

```python
import numpy as np
import ml_dtypes
from contextlib import ExitStack
import concourse.bass as bass
import concourse.mybir as mybir
from concourse.bass_utils import run_bass_kernel_spmd

F32 = mybir.dt.float32
BF16 = mybir.dt.bfloat16
I32 = mybir.dt.int32
AF = mybir.ActivationFunctionType
ALU = mybir.AluOpType
AX = mybir.AxisListType

NCORES = 8
SEQ = 2048
D = 1024
T = 512
EPS = 1e-6
IN_W = 5384
DFF = 2816
NEG = -30000.0
LN16 = float(np.log(16.0))
SINSC = float(2 * np.pi * (1 - 1e-6))


class Res:
    __slots__ = ("name", "w", "rs", "excl")

    def __init__(self, name, excl=False):
        self.name = name
        self.w = []
        self.rs = []
        self.excl = excl


class Acc:
    __slots__ = ("eng", "seq", "ev")

    def __init__(self, eng, seq, ev=None):
        self.eng = eng
        self.seq = seq
        self.ev = ev


class Sync:
    ENGS = ("pe", "dve", "act", "pool", "sp")

    def __init__(self, nc, stack, n_dma_sems=24):
        self.nc = nc
        self.eng = {"pe": nc.tensor, "dve": nc.vector, "act": nc.scalar,
                    "pool": nc.gpsimd, "sp": nc.sync}
        self.sem = {}
        self.cnt = {}
        for e in self.ENGS:
            self.sem[e] = stack.enter_context(nc.semaphore("s_" + e))
            self.cnt[e] = 0
        self.dma_sems = {"sp": [], "pool": []}
        self.dma_last = {}
        for q, n in (("sp", n_dma_sems), ("pool", 16)):
            for i in range(n):
                key = "dma_%s%d" % (q, i)
                self.sem[key] = stack.enter_context(nc.semaphore("s_" + key))
                self.cnt[key] = 0
                self.dma_sems[q].append(key)
                self.dma_last[key] = None
        self.dma_rr = {"sp": 0, "pool": 0}
        self.known = {e: {} for e in self.ENGS}
        self.seq = {e: 0 for e in self.ENGS}
        self.last_inst = {e: None for e in self.ENGS}
        self.inc_log = {e: [] for e in self.ENGS}
        self.n_wait = 0
        self.n_inc = 0
        self.n_inst = 0

    def _materialise(self, acc):
        if acc.ev is not None:
            return acc.ev
        e = acc.eng
        log = self.inc_log[e]
        lo, hi = 0, len(log)
        while lo < hi:
            mid = (lo + hi) // 2
            if log[mid][0] >= acc.seq:
                hi = mid
            else:
                lo = mid + 1
        if lo < len(log):
            _, val, clock = log[lo]
            acc.ev = (e, val, clock)
            return acc.ev
        inst, seq, has_inc = self.last_inst[e]
        assert seq >= acc.seq and not has_inc
        inst.then_inc(self.sem[e], 1)
        self.n_inc += 1
        self.cnt[e] += 1
        clock = dict(self.known[e])
        clock[e] = self.cnt[e]
        log.append((seq, self.cnt[e], clock))
        self.last_inst[e] = (inst, seq, True)
        acc.ev = (e, self.cnt[e], clock)
        return acc.ev

    def _wait(self, e, acc):
        key, val, clock = self._materialise(acc)
        kn = self.known[e]
        if kn.get(key, 0) >= val:
            return
        self.eng[e].wait_ge(self.sem[key], val)
        self.n_wait += 1
        for k, v in clock.items():
            if kn.get(k, 0) < v:
                kn[k] = v
        if kn.get(key, 0) < val:
            kn[key] = val

    STRICT = True

    def _dep(self, e, acc, raw):
        if acc.eng == e:
            if e == "pe" or not (raw or self.STRICT):
                return
        self._wait(e, acc)

    def _deps(self, e, reads, writes):
        for r in reads:
            for a in r.w:
                self._dep(e, a, True)
        for w in writes:
            for a in w.w:
                self._dep(e, a, False if a.eng == e else True)
            for a in w.rs:
                self._dep(e, a, False)

    def op(self, e, fn, reads=(), writes=()):
        ex = [r for r in reads if r.excl]
        if ex:
            reads = [r for r in reads if not r.excl]
            writes = list(writes) + ex
        self._deps(e, reads, writes)
        inst = fn()
        self.n_inst += 1
        self.seq[e] += 1
        s = self.seq[e]
        self.last_inst[e] = (inst, s, False)
        acc = Acc(e, s)
        for r in reads:
            r.rs.append(acc)
        for w in writes:
            w.w = [acc]
            w.rs = []
        return inst

    def dma(self, q, pairs, reads=(), writes=()):
        self._deps(q, reads, writes)
        accs = []
        for (out, in_) in pairs:
            key = self.dma_sems[q][self.dma_rr[q]]
            self.dma_rr[q] = (self.dma_rr[q] + 1) % len(self.dma_sems[q])
            prev = self.dma_last[key]
            if prev is not None:
                self._wait(q, prev)
            inst = self.eng[q].dma_start(out=out, in_=in_)
            inst.then_inc(self.sem[key], 16)
            self.n_inst += 1
            self.cnt[key] += 16
            acc = Acc(key, 0, (key, self.cnt[key], dict(self.known[q])))
            self.dma_last[key] = acc
            accs.append(acc)
        for r in reads:
            r.rs.extend(accs)
        for w in writes:
            w.w = list(accs)
            w.rs = []
        return accs

    def wait_all(self, e, resources):
        for r in resources:
            for a in r.w:
                self._wait(e, a)
            for a in r.rs:
                self._wait(e, a)


CB_IDENT, CB_MASKT, CB_PM, CB_MCUR, CB_MPREV, CB_ONES, CB_ONESA, CB_ONESB = 0, 128, 256, 384, 896, 1408, 1536, 1664
NCB = 1792
CF_IDENT, CF_INVF, CF_HALF, CF_QUART, CF_SCS, CF_BIS, CF_PAR, CF_EYE4, CF_ONES4, CF_BSEL = 0, 128, 129, 130, 131, 132, 133, 261, 265, 393
NCF = 393 + 512


def _consts():
    cb = np.zeros((128, NCB), np.float32)
    cb[:, CB_IDENT:CB_IDENT + 128] = np.eye(128)
    i = np.arange(128)[:, None]
    j = np.arange(128)[None, :]
    cb[:, CB_MASKT:CB_MASKT + 128] = (j >= i)
    p = np.arange(128)
    partner = np.where((p % 64) < 32, p + 32, p - 32)
    pm = np.zeros((128, 128), np.float32)
    pm[partner, p] = 1.0
    cb[:, CB_PM:CB_PM + 128] = pm
    mcur = np.where(j >= i, 0.0, NEG)
    mprev = np.where(j < i, 0.0, NEG)
    cb[:, CB_MCUR:CB_MCUR + 512] = np.tile(mcur, (1, 4))
    cb[:, CB_MPREV:CB_MPREV + 512] = np.tile(mprev, (1, 4))
    cb[:, CB_ONES:CB_ONES + 128] = 1.0
    cb[:, CB_ONESA:CB_ONESA + 64] = 1.0
    cb[:, CB_ONESB + 64:CB_ONESB + 128] = 1.0
    cf = np.zeros((128, NCF), np.float32)
    cf[:, CF_IDENT:CF_IDENT + 128] = np.eye(128)
    inv = 10000.0 ** (-np.arange(0, 64, 2, dtype=np.float32) / 64.0)
    d = (p % 64) % 32
    cf[:, CF_INVF] = inv[d] / (2 * np.pi)
    cf[:, CF_HALF] = 0.5
    cf[:, CF_QUART] = 0.25
    sgn = np.where((p % 64) < 32, -1.0, 1.0)
    cf[:, CF_SCS] = sgn * SINSC
    cf[:, CF_BIS] = -sgn * SINSC / 2
    k8 = np.arange(8)[:, None]
    cf[0:8, CF_PAR:CF_PAR + 128] = ((p[None, :] >= 64) == (k8 % 2 == 1))
    cf[0:4, CF_EYE4:CF_EYE4 + 4] = np.eye(4)
    cf[0:4, CF_ONES4:CF_ONES4 + 128] = 1.0
    col = np.arange(512)[None, :]
    cf[0:8, CF_BSEL:CF_BSEL + 512] = ((col // 128) == (k8 // 2))
    return cb.astype(ml_dtypes.bfloat16), cf.astype(np.float32)


SL_U = 0
SL_QK = 8
SL_QA = 24
SL_KA = 32
SL_CAT = 34
SL_PT = 50
NSLAB = 58
NWB = 3
NFT = 5


def build(nseq=2, ntile=4, dbg=None, stop=None, nomem=False):
    nc = bass.Bass("TRN2", target_bir_lowering=False)
    dt = lambda n, s, d, k="ExternalInput": nc.dram_tensor(n, list(s), d, kind=k).ap()
    x_d = dt("x", [2, SEQ, D], F32)
    mem_d = dt("mem", [2, 256, D], F32)
    pos_d = dt("pos", [2, SEQ], I32)
    w_in_d = dt("w_in", [D, IN_W], F32)
    w_out_d = dt("w_out", [2 * D, D], F32)
    w_xq_d = dt("w_xq", [D, D], F32)
    w_xkv_d = dt("w_xkv", [D, 2 * D], F32)
    w_xo_d = dt("w_xo", [D, D], F32)
    w_gu_d = dt("w_gu", [D, 2 * DFF], F32)
    w_dn_d = dt("w_dn", [DFF, D], F32)
    colp_d = dt("colp", [128, 128], F32)
    rowg_d = dt("rowg", [3, D], F32)
    gb_d = dt("gb", [4, 2], F32)
    sk_d = dt("sk", [8, 2], F32)
    cstb_d = dt("cstb", [128, NCB], BF16)
    cstf_d = dt("cstf", [128, NCF], F32)
    y_d = dt("y", [2, SEQ, D], F32, "ExternalOutput")
    dbg_items = []
    if dbg:
        dbgf_d = dt("dbgf", [128, 32768], F32, "ExternalOutput")
        dbgb_d = dt("dbgb", [128, 65536], BF16, "ExternalOutput")
    dbg_off = {"f": 0, "b": 0}
    out_accs = []

    w_in_v = w_in_d.rearrange("(k p) n -> p k n", p=128)
    w_out_v = w_out_d.rearrange("(k p) n -> p k n", p=128)
    w_xq_v = w_xq_d.rearrange("(k p) n -> p k n", p=128)
    w_xkv_v = w_xkv_d.rearrange("(k p) n -> p k n", p=128)
    w_xo_v = w_xo_d.rearrange("(k p) n -> p k n", p=128)
    w_gu_v = w_gu_d.rearrange("(k p) n -> p k n", p=128)
    w_dn_v = w_dn_d.rearrange("(k p) n -> p k n", p=128)

    with ExitStack() as st:
        S = Sync(nc, st)
        V, A, PE = nc.vector, nc.scalar, nc.tensor
        sb = lambda n, s, d: st.enter_context(nc.sbuf_tensor("sb_" + n, list(s), d))
        dve = lambda fn, r=(), w=(): S.op("dve", fn, r, w)
        act = lambda fn, r=(), w=(): S.op("act", fn, r, w)
        pe = lambda fn, r=(), w=(): S.op("pe", fn, r, w)

        cb = sb("cstb", [128, NCB], BF16)
        cf = sb("cstf", [128, NCF], F32)
        rcst = Res("cst")
        ident = cb[:, CB_IDENT:CB_IDENT + 128]
        maskT = cb[:, CB_MASKT:CB_MASKT + 128]
        Pm = cb[:, CB_PM:CB_PM + 128]
        mcur4 = cb[:, CB_MCUR:CB_MCUR + 512]
        mprev4 = cb[:, CB_MPREV:CB_MPREV + 512]
        ones_bf = cb[:, CB_ONES:CB_ONES + 128]
        onesA = cb[:, CB_ONESA:CB_ONESA + 128]
        onesB = cb[:, CB_ONESB:CB_ONESB + 128]
        identf = cf[:, CF_IDENT:CF_IDENT + 128]

        banks = [st.enter_context(nc.psum_tensor("bk%d" % i, [128, 512], F32)) for i in range(8)]
        rbank = [Res("bk%d" % i, excl=True) for i in range(8)]
        bank_rr = [0]

        def nb():
            i = bank_rr[0]
            bank_rr[0] = (i + 1) % 8
            return banks[i], rbank[i]

        def bfv(bk):
            return bk[:].bitcast(BF16).rearrange("p (a b) -> p a b", b=128)

        SL = sb("SL", [128, NSLAB, 512], BF16)
        rsl = [Res("sl%d" % i) for i in range(NSLAB)]
        h = sb("h", [128, 4, D], F32)
        rh = [Res("h%d" % c) for c in range(4)]
        TOK = sb("tok", [128, 8256], BF16)
        og = TOK[:, 0:4096].rearrange("p (c d) -> p c d", d=1024)
        vx = TOK[:, 4096:4096 + 4112].rearrange("p (c h e) -> p c h e", h=4, e=257)
        abuf = TOK[:, 0:8192].bitcast(F32).rearrange("p (c d) -> p c d", d=1024)
        rog = [Res("og%d" % c) for c in range(4)]
        rvx = [Res("vx%d" % c) for c in range(4)]
        rab = [[rog[0], rog[1]], [rog[2], rog[3]], [rvx[0], rvx[1]], [rvx[1], rvx[2], rvx[3]]]
        wbuf = [sb("wb%d" % i, [128, 8, 512], BF16) for i in range(NWB)]
        rwb = [Res("wb%d" % i) for i in range(NWB)]
        ft = [sb("ft%d" % i, [128, 520], F32) for i in range(NFT)]
        rft = [Res("ft%d" % i) for i in range(NFT)]
        ft_rr = [0]

        def nft():
            i = ft_rr[0]
            ft_rr[0] = (i + 1) % NFT
            return ft[i], rft[i]

        utok = [sb("utok%d" % i, [128, D], BF16) for i in range(2)]
        rutok = [Res("utok%d" % i) for i in range(2)]
        junk = [sb("junk%d" % i, [128, D], BF16) for i in range(1)]
        rjunk = [Res("junk%d" % i) for i in range(1)]
        rr2 = {"utok": 0, "junk": 0, "qb": 0, "hmn": 0, "rd": 0, "vpad": 0}

        def ring(name, n):
            i = rr2[name]
            rr2[name] = (i + 1) % n
            return i

        cosT = sb("cosT", [128, 512], F32)
        sinS = sb("sinS", [128, 512], F32)
        rcos, rsin = Res("cos"), Res("sin")
        qb = [sb("qb%d" % i, [128, 512], BF16) for i in range(2)]
        rqb = [Res("qb%d" % i) for i in range(2)]
        GX = [sb("gx%d" % i, [4, 512], F32) for i in range(3)]
        rgx = [Res("gx%d" % i) for i in range(3)]
        gsm = sb("gsm", [4, 64], F32)
        rgsm = Res("gsm")
        mall = sb("mall", [4, 8], F32)
        rmall = Res("mall")
        Rm = sb("Rm", [4, 16], F32)
        rRm = Res("Rm")
        wtok = sb("wtok", [128, 48], F32)
        rwtok = Res("wtok")
        Cst = sb("Cst", [128, 4, 2, 257], F32)
        rC = [Res("C%d" % i) for i in range(4)]
        Cb = sb("Cb", [128, 4, 2, 257], BF16)
        rCb = [Res("Cb%d" % i) for i in range(4)]
        PT = sb("PT", [128, 4, 128], BF16)
        rPT = [Res("PT%d" % i) for i in range(4)]
        kw = sb("kw", [128, 4, 256], BF16)
        rkw = [Res("kw%d" % i) for i in range(4)]
        nd = sb("nd", [128, 4, 257], F32)
        rnd = [Res("nd%d" % i) for i in range(4)]
        fin = sb("fin", [128, 32], F32)
        rfin = Res("fin")
        hmn = [sb("hmn%d" % i, [128, D], BF16) for i in range(1)]
        rhmn = [Res("hmn%d" % i) for i in range(1)]
        tails = sb("tails", [128, 16, 3], F32)
        rtails = [Res("tail%d" % i) for i in range(16)]
        vtok = sb("vtok", [128, 4, 128], BF16)
        rvtok = [Res("vtok%d" % c) for c in range(4)]
        vpad = [sb("vpad%d" % i, [128, 2, 2, 128], BF16) for i in range(3)]
        rvpad = [Res("vpad%d" % i) for i in range(3)]
        prevK = sb("prevK", [128, 2, 128], BF16)
        rprevK = Res("prevK")
        rd = [sb("rd%d" % i, [128, 512], F32) for i in range(2)]
        rrd = [Res("rd%d" % i) for i in range(2)]
        KmT = sb("KmT", [128, 8, 256], BF16)
        rKm = Res("KmT")
        Vm = sb("Vm", [128, 2, D], BF16)
        rVm = Res("Vm")
        memT = sb("memT", [128, 8, 256], BF16)
        rmemT = Res("memT")
        grow = sb("grow", [128, 3, D], F32)
        rgrow = Res("grow")
        colst = sb("colst", [128, 128], F32)
        colp = sb("colp", [128, 128], F32)
        rcolst, rcolp = Res("colst"), Res("colp")
        gbt = sb("gbt", [4, 4], F32)
        rgbt = Res("gbt")
        skt = sb("skt", [8, 4], F32)
        rskt = Res("skt")
        Asink = sb("Asink", [8, 2, 128], F32)
        rAs = Res("Asink")
        nsm = sb("nsm", [128, 16], F32)
        rnsm = Res("nsm")
        ry = Res("y")
        rdbg = Res("dbg")

        convw = lambda j, o: colp[:, j * 16 + o: j * 16 + o + 1]
        g_pre1 = colp[:, 64:72]
        g_xpre = colp[:, 72:80]
        g_fpre = colp[:, 80:88]
        g_mem = colp[:, 88:96]
        g_mls = colp[:, 96:104]

        def dump(name, ap, res, kind):
            if not dbg or name not in dbg:
                return
            shp = list(ap.shape)
            n = int(np.prod(shp[1:]))
            o = dbg_off[kind]
            dbg_off[kind] += n
            dst = (dbgf_d if kind == "f" else dbgb_d)[0:shp[0], o:o + n]
            if len(shp) == 3:
                dst = dst.rearrange("p (a b) -> p a b", b=shp[2])
            elif len(shp) == 4:
                dst = dst.rearrange("p (a b c) -> p a b c", b=shp[2], c=shp[3])
            out_accs.extend(S.dma("sp", [(dst, ap)], reads=list(res), writes=[rdbg]))
            dbg_items.append((name, kind, o, shp))

        S.dma("sp", [(cb[:], cstb_d[:, :]), (cf[:], cstf_d[:, :])], writes=[rcst])
        S.dma("sp", [(colst[:], colp_d[:, :])], writes=[rcolst])
        S.dma("sp", [(grow[:, i, :], rowg_d[i:i + 1, :].partition_broadcast(128)) for i in range(3)], writes=[rgrow])
        S.dma("sp", [(gbt[0:4, 0:2], gb_d[:, :])], writes=[rgbt])
        S.dma("sp", [(skt[0:8, 0:2], sk_d[:, :])], writes=[rskt])
        bk, rbk = nb()
        pe(lambda: PE.transpose(bk[:, 0:128], colst[:, :], identf), r=[rcolst, rcst], w=[rbk])
        dve(lambda: V.tensor_copy(colp[:], bk[:, 0:128]), r=[rbk], w=[rcolp])
        dve(lambda: V.tensor_scalar(gbt[0:4, 2:3], gbt[0:4, 1:2], -1.0, None, ALU.mult), r=[rgbt], w=[rgbt])
        act(lambda: A.activation(skt[0:8, 2:4], skt[0:8, 0:2], AF.Exp), r=[rskt], w=[rskt])
        for g in range(2):
            dve(lambda: V.tensor_scalar(Asink[0:8, g, :], cf[0:8, CF_PAR:CF_PAR + 128], skt[0:8, 2 + g:3 + g], None, ALU.mult),
                r=[rskt, rcst], w=[rAs])
        for i in range(3):
            dve(lambda: V.memset(vpad[i][:], 0.0), w=[rvpad[i]])

        def seg(view, k0, nk, c0, ncol, dcol=0):
            return (lambda wb: wb[:, 0:nk, dcol:dcol + ncol], view[:, k0:k0 + nk, c0:c0 + ncol])

        specs = []
        for s in range(nseq):
            for i in range(4):
                specs.append([seg(w_xkv_v, 0, 8, i * 512, 512)])
            for t in range(ntile):
                for i in range(8):
                    specs.append([seg(w_in_v, 0, 8, i * 512, 512)])
                blk8 = [seg(w_in_v, 0, 8, 4096, 8, 0), seg(w_in_v, 0, 8, 5256, 128, 8)]
                for g in range(2):
                    for r_ in range(2):
                        blk8.append(seg(w_in_v, 0, 8, 5128 + g * 64, 64, 136 + g * 128 + r_ * 64))
                specs.append(blk8)
                specs.append([seg(w_in_v, 0, 8, 4104, 512)])
                specs.append([seg(w_in_v, 0, 8, 4616, 512)])
                for half in range(2):
                    for kh in range(2):
                        specs.append([seg(w_out_v, kh * 8, 8, half * 512, 512)])
                for half in range(2):
                    specs.append([seg(w_xq_v, 0, 8, half * 512, 512)])
                for half in range(2):
                    specs.append([seg(w_xo_v, 0, 8, half * 512, 512)])
                for i in range(6):
                    ncol = 512 if i < 5 else 256
                    specs.append([seg(w_gu_v, 0, 8, i * 512, ncol)])
                    specs.append([seg(w_gu_v, 0, 8, DFF + i * 512, ncol)])
                for half in range(2):
                    for (k0, nk) in ((0, 8), (8, 8), (16, 6)):
                        specs.append([seg(w_dn_v, k0, nk, half * 512, 512)])
        wst = {"issued": 0, "taken": 0}

        def getw():
            n = wst["taken"]
            lim = min(len(specs), n + NWB)
            while wst["issued"] < lim:
                m = wst["issued"]
                i = m % NWB
                S.dma("pool", [(f(wbuf[i]), src) for (f, src) in specs[m]], writes=[rwb[i]])
                wst["issued"] += 1
            wst["taken"] += 1
            return wbuf[n % NWB], rwb[n % NWB]

        def rstd_from_ss(n):
            dve(lambda: V.tensor_scalar(nsm[:, 4:4 + n], nsm[:, 0:n], 1.0 / D, EPS, ALU.mult, ALU.add), r=[rnsm], w=[rnsm])
            act(lambda: A.activation(nsm[:, 4:4 + n], nsm[:, 4:4 + n], AF.Sqrt), r=[rnsm], w=[rnsm])
            dve(lambda: V.reciprocal(nsm[:, 8:8 + n], nsm[:, 4:4 + n]), r=[rnsm], w=[rnsm])

        def prenorm(srcs, gcol, dsts):
            n = len(srcs)
            for c, (ap, rs) in enumerate(srcs):
                ji = ring("junk", 1)
                act(lambda: A.activation(junk[ji][:], ap, AF.Square, accum_out=nsm[:, c:c + 1]), r=rs, w=[rjunk[ji], rnsm])
            rstd_from_ss(n)
            for c, (ap, rs) in enumerate(srcs):
                ui = ring("utok", 2)
                act(lambda: A.activation(utok[ui][:], ap, AF.Copy, scale=nsm[:, 8 + c:9 + c]), r=rs + [rnsm], w=[rutok[ui]])
                bk, rbk = nb()
                bv = bfv(bk)
                for k in range(8):
                    pe(lambda: PE.transpose(bv[:, k, :], utok[ui][:, k * 128:(k + 1) * 128], ident), r=[rutok[ui], rcst], w=[rbk])
                dap, drs = dsts[c]
                dve(lambda: V.tensor_tensor(dap, bv[:, :, :], gcol.unsqueeze(2).broadcast_to([128, 8, 128]), ALU.mult),
                    r=[rbk, rcolp], w=drs)

        def tok_gemm(kslabs, nblk_per_half, nks):
            for half in range(2):
                bks = [nb() for _ in range(4)]
                ki = 0
                for bi in range(nblk_per_half):
                    wb, rw = getw()
                    nk = nks[bi]
                    for c in range(4):
                        for kk in range(nk):
                            first = (bi == 0 and kk == 0)
                            last = (bi == nblk_per_half - 1 and kk == nk - 1)
                            sl = kslabs[ki + kk]
                            pe(lambda: PE.matmul(bks[c][0][:, :], SL[:, sl, c * 128:(c + 1) * 128], wb[:, kk, 0:512], start=first, stop=last),
                               r=[rsl[sl], rw], w=[bks[c][1]])
                    ki += nk
                for c in range(4):
                    act(lambda: A.copy(abuf[:, c, half * 512:(half + 1) * 512], bks[c][0][:, :]), r=[bks[c][1]], w=rab[c])

        def postnorm(gi):
            for c in range(4):
                ji = ring("junk", 1)
                act(lambda: A.activation(junk[ji][:], abuf[:, c, :], AF.Square, accum_out=nsm[:, c:c + 1]), r=rab[c], w=[rjunk[ji], rnsm])
            rstd_from_ss(4)
            for c in range(4):
                for hf in range(2):
                    t1, rt1 = nft()
                    sl_ = slice(hf * 512, (hf + 1) * 512)
                    dve(lambda: V.scalar_tensor_tensor(t1[:, 0:512], abuf[:, c, sl_], nsm[:, 8 + c:9 + c], grow[:, gi, sl_], ALU.mult, ALU.mult),
                        r=rab[c] + [rnsm, rgrow], w=[rt1])
                    dve(lambda: V.tensor_tensor(h[:, c, sl_], h[:, c, sl_], t1[:, 0:512], ALU.add), r=[rh[c], rt1], w=[rh[c]])

        def mem_stage(s):
            S.dma("sp", [(h[:, 0:2, :], mem_d[s].rearrange("(c p) d -> p c d", p=128))], writes=[rh[0], rh[1]])
            prenorm([(h[:, c, :], [rh[c]]) for c in range(2)], g_mem,
                    [(memT[:, :, c * 128:(c + 1) * 128], [rmemT]) for c in range(2)])
            for b_ in range(2):
                wb, rw = getw()
                for j in range(4):
                    oc = b_ * 4 + j
                    bk, rbk = nb()
                    for k in range(8):
                        pe(lambda: PE.matmul(bk[:, 0:256], wb[:, k, j * 128:(j + 1) * 128], memT[:, k, :], start=(k == 0), stop=(k == 7)),
                           r=[rw, rmemT], w=[rbk])
                    act(lambda: A.copy(KmT[:, oc, :], bk[:, 0:256]), r=[rbk], w=[rKm])
            for b_ in range(2):
                wb, rw = getw()
                for mc in range(2):
                    bk, rbk = nb()
                    for k in range(8):
                        pe(lambda: PE.matmul(bk[:, :], memT[:, k, mc * 128:(mc + 1) * 128], wb[:, k, 0:512], start=(k == 0), stop=(k == 7)),
                           r=[rw, rmemT], w=[rbk])
                    act(lambda: A.copy(Vm[:, mc, b_ * 512:(b_ + 1) * 512], bk[:, :]), r=[rbk], w=[rVm])

        def rope_tables(s, t0):
            pi_, rpi = nft()
            yv, ryv = nft()
            fr, rfr = nft()
            posi = pi_[:, 0:512].bitcast(I32)
            S.dma("sp", [(posi, pos_d[s:s + 1, t0:t0 + 512].partition_broadcast(128))], writes=[rpi])
            dve(lambda: V.tensor_copy(yv[:, 0:512], posi), r=[rpi], w=[ryv])
            dve(lambda: V.tensor_scalar(yv[:, 0:512], yv[:, 0:512], cf[:, CF_INVF:CF_INVF + 1], cf[:, CF_HALF:CF_HALF + 1], ALU.mult, ALU.add),
                r=[ryv, rcst], w=[ryv])
            for which in range(2):
                if which == 1:
                    dve(lambda: V.tensor_scalar(yv[:, 0:512], yv[:, 0:512], cf[:, CF_QUART:CF_QUART + 1], None, ALU.add), r=[ryv, rcst], w=[ryv])
                dve(lambda: V.tensor_copy(posi, yv[:, 0:512]), r=[ryv], w=[rpi])
                dve(lambda: V.tensor_tensor(fr[:, 0:512], yv[:, 0:512], posi, ALU.subtract), r=[ryv, rpi], w=[rfr])
                dve(lambda: V.scalar_tensor_tensor(fr[:, 0:512], fr[:, 0:512], 0.0, fr[:, 0:512], ALU.is_lt, ALU.add), r=[rfr], w=[rfr])
                if which == 0:
                    act(lambda: A.activation(sinS[:], fr[:, 0:512], AF.Sin, scale=cf[:, CF_SCS:CF_SCS + 1], bias=cf[:, CF_BIS:CF_BIS + 1]),
                        r=[rfr, rcst], w=[rsin])
                else:
                    act(lambda: A.activation(cosT[:], fr[:, 0:512], AF.Sin, scale=SINSC, bias=-SINSC / 2), r=[rfr], w=[rcos])

        def rope(bk, rbk, dst):
            if stop == "B3d":
                qi = ring("qb", 2)
                act(lambda: A.copy(qb[qi][:], bk[:, :]), r=[rbk], w=[rqb[qi]])
                b2, rb2 = nb()
                pe(lambda: PE.matmul(b2[:, :], Pm, qb[qi][:], start=True, stop=True), r=[rcst, rqb[qi]], w=[rb2])
                act(lambda: A.copy(SL[:, dst, :], b2[:, :]), r=[rb2], w=[rsl[dst]])
                return
            if stop == "B3e":
                t1, rt1 = nft()
                dve(lambda: V.tensor_tensor(t1[:, 0:512], bk[:, :], cosT[:], ALU.mult), r=[rbk, rcos], w=[rt1])
                dve(lambda: V.tensor_tensor(SL[:, dst, :], t1[:, 0:512], sinS[:], ALU.add), r=[rt1, rsin], w=[rsl[dst]])
                return
            qi = ring("qb", 2)
            act(lambda: A.copy(qb[qi][:], bk[:, :]), r=[rbk], w=[rqb[qi]])
            b2, rb2 = nb()
            pe(lambda: PE.matmul(b2[:, :], Pm, qb[qi][:], start=True, stop=True), r=[rcst, rqb[qi]], w=[rb2])
            t1, rt1 = nft()
            t2, rt2 = nft()
            if stop == "B3g":
                dve(lambda: V.tensor_tensor(t1[:, 0:512], bk[:, :], cosT[:], ALU.mult), r=[rbk, rcos], w=[rt1])
                dve(lambda: V.tensor_tensor(t2[:, 0:512], b2[:, :], sinS[:], ALU.mult), r=[rb2, rsin], w=[rt2])
                dve(lambda: V.tensor_copy(SL[:, dst, :], t1[:, 0:512]), r=[rt1, rt2], w=[rsl[dst]])
                return
            if stop == "B3h":
                dve(lambda: V.tensor_tensor(t2[:, 0:512], b2[:, :], sinS[:], ALU.mult), r=[rb2, rsin], w=[rt2])
                dve(lambda: V.tensor_tensor(SL[:, dst, :], t2[:, 0:512], cosT[:], ALU.add), r=[rt2, rcos], w=[rsl[dst]])
                return
            dve(lambda: V.tensor_tensor(t1[:, 0:512], bk[:, :], cosT[:], ALU.mult), r=[rbk, rcos], w=[rt1])
            dve(lambda: V.tensor_tensor(t2[:, 0:512], b2[:, :], sinS[:], ALU.mult), r=[rb2, rsin], w=[rt2])
            dve(lambda: V.tensor_tensor(SL[:, dst, :], t1[:, 0:512], t2[:, 0:512], ALU.add), r=[rt1, rt2], w=[rsl[dst]])

        def tile(s, t):
            t0 = t * T
            first_tile = (t == 0)
            S.dma("sp", [(h[:, :, :], x_d[s, t0:t0 + T, :].rearrange("(c p) d -> p c d", p=128))], writes=rh)

            def fin_tile():
                out_accs.extend(S.dma("sp", [(y_d[s, t0:t0 + T, :].rearrange("(c p) d -> p c d", p=128), h[:, :, :])], reads=rh, writes=[ry]))
            if stop == "L":
                return fin_tile()
            rope_tables(s, t0)
            if stop == "R":
                return fin_tile()
            prenorm([(h[:, c, :], [rh[c]]) for c in range(4)], g_pre1,
                    [(SL[:, SL_U:SL_U + 8, c * 128:(c + 1) * 128], rsl[SL_U:SL_U + 8]) for c in range(4)])
            dump("uT", SL[:, SL_U:SL_U + 8, :], rsl[SL_U:SL_U + 8], "b")
            if stop == "A":
                return fin_tile()

            for b_ in range(4):
                wb, rw = getw()
                for j in range(4):
                    oc = b_ * 4 + j
                    bk, rbk = nb()
                    for k in range(8):
                        pe(lambda: PE.matmul(bk[:, :], wb[:, k, j * 128:(j + 1) * 128], SL[:, SL_U + k, :], start=(k == 0), stop=(k == 7)),
                           r=[rw, rsl[SL_U + k]], w=[rbk])
                    cbf, rcb = nft()
                    acc, racc = nft()
                    act(lambda: A.copy(cbf[:, 3:515], bk[:, :]), r=[rbk], w=[rcb])
                    if first_tile:
                        dve(lambda: V.memset(cbf[:, 0:3], 0.0), w=[rcb])
                    else:
                        dve(lambda: V.tensor_copy(cbf[:, 0:3], tails[:, oc, :]), r=[rtails[oc]], w=[rcb])
                    act(lambda: A.activation(acc[:, 0:512], bk[:, :], AF.Copy, scale=convw(3, oc)), r=[rbk, rcolp], w=[racc])
                    dve(lambda: V.tensor_copy(tails[:, oc, :], cbf[:, 512:515]), r=[rcb], w=[rtails[oc]])
                    for jj in range(3):
                        dve(lambda: V.scalar_tensor_tensor(acc[:, 0:512], cbf[:, jj:jj + 512], convw(jj, oc), acc[:, 0:512], ALU.mult, ALU.add),
                            r=[rcb, racc, rcolp], w=[racc])
                    act(lambda: A.activation(SL[:, SL_QK + oc, :], acc[:, 0:512], AF.Silu), r=[racc], w=[rsl[SL_QK + oc]])
            dump("qkT", SL[:, SL_QK:SL_QK + 16, :], rsl[SL_QK:SL_QK + 16], "b")
            if stop == "B1":
                return fin_tile()
            for c in range(4):
                dve(lambda: V.memset(vx[:, c, :, 256:257], 1.0), w=[rvx[c]])
            for b_ in range(2):
                wb, rw = getw()
                for c in range(4):
                    bk, rbk = nb()
                    for k in range(8):
                        pe(lambda: PE.matmul(bk[:, :], SL[:, SL_U + k, c * 128:(c + 1) * 128], wb[:, k, 0:512], start=(k == 0), stop=(k == 7)),
                           r=[rw, rsl[SL_U + k]], w=[rbk])
                    act(lambda: A.copy(vx[:, c, 2 * b_:2 * b_ + 2, 0:256], bk[:, :].rearrange("p (h e) -> p h e", e=256)), r=[rbk], w=[rvx[c]])
            for b_ in range(2):
                wb, rw = getw()
                for c in range(4):
                    bk, rbk = nb()
                    for k in range(8):
                        pe(lambda: PE.matmul(bk[:, :], SL[:, SL_U + k, c * 128:(c + 1) * 128], wb[:, k, 0:512], start=(k == 0), stop=(k == 7)),
                           r=[rw, rsl[SL_U + k]], w=[rbk])
                    act(lambda: A.activation(og[:, c, b_ * 512:(b_ + 1) * 512], bk[:, :], AF.Sigmoid), r=[rbk], w=[rog[c]])
            dump("og", og[:, :, :], rog, "b")
            if stop == "B2":
                return fin_tile()
            wb, rw = getw()
            bkI, rbkI = nb()
            bkF, rbkF = nb()
            for k in range(8):
                pe(lambda: PE.matmul(bkI[0:4, :], wb[:, k, 0:4], SL[:, SL_U + k, :], start=(k == 0), stop=(k == 7)), r=[rw, rsl[SL_U + k]], w=[rbkI])
            for k in range(8):
                pe(lambda: PE.matmul(bkF[0:4, :], wb[:, k, 4:8], SL[:, SL_U + k, :], start=(k == 0), stop=(k == 7)), r=[rw, rsl[SL_U + k]], w=[rbkF])
            gates(bkI, rbkI, bkF, rbkF, first_tile)
            if stop == "B3":
                return fin_tile()
            for c in range(4):
                bk, rbk = nb()
                for k in range(8):
                    pe(lambda: PE.matmul(bk[:, 0:128], SL[:, SL_U + k, c * 128:(c + 1) * 128], wb[:, k, 8:136], start=(k == 0), stop=(k == 7)),
                       r=[rw, rsl[SL_U + k]], w=[rbk])
                act(lambda: A.copy(vtok[:, c, :], bk[:, 0:128]), r=[rbk], w=[rvtok[c]])
            if stop == "B3a":
                return fin_tile()
            if not first_tile:
                for g in range(2):
                    dve(lambda: V.tensor_copy(prevK[:, g, :], SL[:, SL_KA + g, 384:512]), r=[rsl[SL_KA + g]], w=[rprevK])
            for g in range(2):
                bk, rbk = nb()
                for k in range(8):
                    pe(lambda: PE.matmul(bk[:, :], wb[:, k, 136 + g * 128:136 + (g + 1) * 128], SL[:, SL_U + k, :], start=(k == 0), stop=(k == 7)),
                       r=[rw, rsl[SL_U + k]], w=[rbk])
                if stop == "B3c":
                    act(lambda: A.copy(SL[:, SL_KA + g, :], bk[:, :]), r=[rbk], w=[rsl[SL_KA + g]])
                else:
                    rope(bk, rbk, SL_KA + g)
            if stop in ("B3b", "B3c", "B3d", "B3e", "B3g", "B3h"):
                return fin_tile()
            for b_ in range(2):
                wb, rw = getw()
                for j in range(4):
                    bk, rbk = nb()
                    for k in range(8):
                        pe(lambda: PE.matmul(bk[:, :], wb[:, k, j * 128:(j + 1) * 128], SL[:, SL_U + k, :], start=(k == 0), stop=(k == 7)),
                           r=[rw, rsl[SL_U + k]], w=[rbk])
                    rope(bk, rbk, SL_QA + b_ * 4 + j)
            dump("qaT", SL[:, SL_QA:SL_QA + 8, :], rsl[SL_QA:SL_QA + 8], "b")
            dump("kaT", SL[:, SL_KA:SL_KA + 2, :], rsl[SL_KA:SL_KA + 2], "b")
            dump("wtok", wtok[:, :], [rwtok], "f")
            if stop == "B4":
                return fin_tile()

            for c in range(4):
                if stop != "C-nomlstm":
                    mlstm_chunk(c)
                if stop != "C-noswa":
                    swa_block(t, c)
            dump("catT", SL[:, SL_CAT:SL_CAT + 16, :], rsl[SL_CAT:SL_CAT + 16], "b")
            if stop == "C":
                return fin_tile()

            tok_gemm(list(range(SL_CAT, SL_CAT + 16)), 2, [8, 8])
            dump("a", abuf[:, :, :], rog + rvx, "f")
            postnorm(0)
            dump("h1", h[:, :, :], rh, "f")
            if stop == "D":
                return fin_tile()

            prenorm([(h[:, c, :], [rh[c]]) for c in range(4)], g_xpre,
                    [(SL[:, SL_U:SL_U + 8, c * 128:(c + 1) * 128], rsl[SL_U:SL_U + 8]) for c in range(4)])
            QX, OX = SL_QK, SL_QK + 8
            for b_ in range(2):
                wb, rw = getw()
                for j in range(4):
                    oc = b_ * 4 + j
                    bk, rbk = nb()
                    for k in range(8):
                        pe(lambda: PE.matmul(bk[:, :], wb[:, k, j * 128:(j + 1) * 128], SL[:, SL_U + k, :], start=(k == 0), stop=(k == 7)),
                           r=[rw, rsl[SL_U + k]], w=[rbk])
                    act(lambda: A.copy(SL[:, QX + oc, :], bk[:, :]), r=[rbk], w=[rsl[QX + oc]])
            for hd in range(4):
                for mc in range(2):
                    bk, rbk = nb()
                    for dc in range(2):
                        pe(lambda: PE.matmul(bk[:, :], KmT[:, hd * 2 + dc, mc * 128:(mc + 1) * 128], SL[:, QX + hd * 2 + dc, :], start=(dc == 0), stop=(dc == 1)),
                           r=[rKm, rsl[QX + hd * 2 + dc]], w=[rbk])
                    act(lambda: A.activation(SL[:, SL_PT + hd * 2 + mc, :], bk[:, :], AF.Exp, scale=1.0 / 16.0), r=[rbk], w=[rsl[SL_PT + hd * 2 + mc]])
            for hd in range(4):
                bkD, rbkD = nb()
                for mc in range(2):
                    pe(lambda: PE.matmul(bkD[:, :], ones_bf, SL[:, SL_PT + hd * 2 + mc, :], start=(mc == 0), stop=(mc == 1)),
                       r=[rcst, rsl[SL_PT + hd * 2 + mc]], w=[rbkD])
                ri = ring("rd", 2)
                dve(lambda: V.reciprocal(rd[ri][:], bkD[:, :]), r=[rbkD], w=[rrd[ri]])
                for ec in range(2):
                    bkO, rbkO = nb()
                    for mc in range(2):
                        pe(lambda: PE.matmul(bkO[:, :], Vm[:, mc, hd * 256 + ec * 128:hd * 256 + (ec + 1) * 128], SL[:, SL_PT + hd * 2 + mc, :], start=(mc == 0), stop=(mc == 1)),
                           r=[rVm, rsl[SL_PT + hd * 2 + mc]], w=[rbkO])
                    dve(lambda: V.tensor_tensor(SL[:, OX + hd * 2 + ec, :], bkO[:, :], rd[ri][:], ALU.mult), r=[rbkO, rrd[ri]], w=[rsl[OX + hd * 2 + ec]])
            tok_gemm(list(range(OX, OX + 8)), 1, [8])
            dump("c", abuf[:, :, :], rog + rvx, "f")
            postnorm(1)
            dump("h2", h[:, :, :], rh, "f")
            if stop == "E":
                return fin_tile()

            prenorm([(h[:, c, :], [rh[c]]) for c in range(4)], g_fpre,
                    [(SL[:, SL_U:SL_U + 8, c * 128:(c + 1) * 128], rsl[SL_U:SL_U + 8]) for c in range(4)])
            HT = SL_QK
            for i in range(6):
                nj = 4 if i < 5 else 2
                wb, rw = getw()
                for j in range(nj):
                    bk, rbk = nb()
                    for k in range(8):
                        pe(lambda: PE.matmul(bk[:, :], wb[:, k, j * 128:(j + 1) * 128], SL[:, SL_U + k, :], start=(k == 0), stop=(k == 7)),
                           r=[rw, rsl[SL_U + k]], w=[rbk])
                    act(lambda: A.activation(SL[:, HT + i * 4 + j, :], bk[:, :], AF.Silu), r=[rbk], w=[rsl[HT + i * 4 + j]])
                wb, rw = getw()
                for j in range(nj):
                    bk, rbk = nb()
                    for k in range(8):
                        pe(lambda: PE.matmul(bk[:, :], wb[:, k, j * 128:(j + 1) * 128], SL[:, SL_U + k, :], start=(k == 0), stop=(k == 7)),
                           r=[rw, rsl[SL_U + k]], w=[rbk])
                    dve(lambda: V.tensor_tensor(SL[:, HT + i * 4 + j, :], bk[:, :], SL[:, HT + i * 4 + j, :], ALU.mult),
                        r=[rbk, rsl[HT + i * 4 + j]], w=[rsl[HT + i * 4 + j]])
            tok_gemm(list(range(HT, HT + 22)), 3, [8, 8, 6])
            dump("f", abuf[:, :, :], rog + rvx, "f")
            postnorm(2)
            out_accs.extend(S.dma("sp", [(y_d[s, t0:t0 + T, :].rearrange("(c p) d -> p c d", p=128), h[:, :, :])], reads=rh, writes=[ry]))

        def gates(bkI, rbkI, bkF, rbkF, first_tile):
            X1, X2, X3 = GX[0], GX[1], GX[2]
            r1, r2, r3 = rgx
            if first_tile:
                dve(lambda: V.memset(mall[0:4, :], 0.0), w=[rmall])
            else:
                dve(lambda: V.tensor_copy(mall[0:4, 0:1], mall[0:4, 4:5]), r=[rmall], w=[rmall])
            act(lambda: A.activation(X1[0:4, :], bkF[0:4, :], AF.Exp, scale=-1.0, bias=gbt[0:4, 2:3]), r=[rbkF, rgbt], w=[r1])
            act(lambda: A.activation(X1[0:4, :], X1[0:4, :], AF.Ln, bias=1.0), r=[r1], w=[r1])
            for c in range(4):
                dve(lambda: V.tensor_tensor_scan(X3[0:4, c * 128:(c + 1) * 128], cf[0:4, CF_ONES4:CF_ONES4 + 128], X1[0:4, c * 128:(c + 1) * 128], 0.0, ALU.mult, ALU.add),
                    r=[r1, rcst], w=[r3])
            dve(lambda: V.scalar_tensor_tensor(X2[0:4, :], bkI[0:4, :], gbt[0:4, 0:1], X3[0:4, :], ALU.add, ALU.add), r=[rbkI, rgbt, r3], w=[r2])
            dve(lambda: V.tensor_reduce(gsm[0:4, 0:4], X2[0:4, :].rearrange("p (a b) -> p a b", b=128), AX.X, ALU.max), r=[r2], w=[rgsm])
            dve(lambda: V.tensor_scalar(gsm[0:4, 4:8], X3[0:4, :].rearrange("p (a b) -> p a b", b=128)[:, :, 127], -1.0, None, ALU.mult), r=[r3], w=[rgsm])
            dve(lambda: V.tensor_tensor_scan(mall[0:4, 1:5], gsm[0:4, 0:4], gsm[0:4, 4:8], mall[0:4, 0:1], ALU.max, ALU.add), r=[rgsm, rmall], w=[rmall])
            dve(lambda: V.tensor_tensor(gsm[0:4, 8:12], mall[0:4, 0:4], gsm[0:4, 0:4], ALU.max), r=[rgsm, rmall], w=[rgsm])
            dve(lambda: V.tensor_scalar(gsm[0:4, 12:16], gsm[0:4, 8:12], -1.0, None, ALU.mult), r=[rgsm], w=[rgsm])
            dve(lambda: V.tensor_scalar(gsm[0:4, 16:20], gsm[0:4, 8:12], -1.0, -LN16, ALU.mult, ALU.add), r=[rgsm], w=[rgsm])
            dve(lambda: V.tensor_tensor(gsm[0:4, 20:24], mall[0:4, 0:4], gsm[0:4, 8:12], ALU.subtract), r=[rgsm, rmall], w=[rgsm])
            act(lambda: A.activation(gsm[0:4, 20:24], gsm[0:4, 20:24], AF.Exp), r=[rgsm], w=[rgsm])
            for c in range(4):
                act(lambda: A.activation(X1[0:4, c * 128:(c + 1) * 128], X2[0:4, c * 128:(c + 1) * 128], AF.Exp, bias=gsm[0:4, 16 + c:17 + c]), r=[r2, rgsm, r1], w=[r1])
                act(lambda: A.activation(X3[0:4, c * 128:(c + 1) * 128], X3[0:4, c * 128:(c + 1) * 128], AF.Exp, bias=gsm[0:4, 12 + c:13 + c]), r=[r3, rgsm], w=[r3])
            bk, rbk = nb()
            for c in range(4):
                pe(lambda: PE.transpose(bk[:, c * 4:(c + 1) * 4], X1[0:4, c * 128:(c + 1) * 128], identf[0:4, 0:4]), r=[r1, rcst], w=[rbk])
                pe(lambda: PE.transpose(bk[:, 16 + c * 4:16 + (c + 1) * 4], X3[0:4, c * 128:(c + 1) * 128], identf[0:4, 0:4]), r=[r3, rcst], w=[rbk])
            dve(lambda: V.tensor_tensor(Rm[0:4, :].rearrange("p (a b) -> p a b", b=4),
                                        gsm[0:4, 20:24].unsqueeze(2).broadcast_to([4, 4, 4]),
                                        cf[0:4, CF_EYE4:CF_EYE4 + 4].unsqueeze(1).broadcast_to([4, 4, 4]), ALU.mult),
                r=[rgsm, rcst], w=[rRm])
            pe(lambda: PE.matmul(bk[:, 32:48], cf[0:4, CF_ONES4:CF_ONES4 + 128], Rm[0:4, :], start=True, stop=True), r=[rRm, rcst], w=[rbk])
            dve(lambda: V.tensor_copy(wtok[:, 0:48], bk[:, 0:48]), r=[rbk], w=[rwtok])

        def mlstm_chunk(c):
            cs = slice(c * 128, (c + 1) * 128)
            Q0, K0 = SL_QK, SL_QK + 8
            bkS, rbkS = nb()
            bkT, rbkT = nb()
            bvT = bfv(bkT)
            for hd in range(4):
                for dc in range(2):
                    pe(lambda: PE.matmul(bkS[:, hd * 128:(hd + 1) * 128], SL[:, K0 + hd * 2 + dc, cs], SL[:, Q0 + hd * 2 + dc, cs], start=(dc == 0), stop=(dc == 1)),
                       r=[rsl[K0 + hd * 2 + dc], rsl[Q0 + hd * 2 + dc]], w=[rbkS])
            for i in range(8):
                pe(lambda: PE.transpose(bvT[:, i, :], SL[:, K0 + i, cs], ident), r=[rsl[K0 + i], rcst], w=[rbkT])
            for hd in range(4):
                wcol = wtok[:, c * 4 + hd:c * 4 + hd + 1]
                dcol = wtok[:, 32 + c * 4 + hd:32 + c * 4 + hd + 1]
                dve(lambda: V.scalar_tensor_tensor(PT[:, hd, :], bkS[:, hd * 128:(hd + 1) * 128], wcol, maskT, ALU.mult, ALU.mult),
                    r=[rbkS, rwtok, rcst], w=[rPT[hd]])
                act(lambda: A.activation(kw[:, hd, :].rearrange("p (a b) -> p a b", b=128), bvT[:, 2 * hd:2 * hd + 2, :], AF.Copy, scale=wcol),
                    r=[rbkT, rwtok], w=[rkw[hd]])
                dve(lambda: V.tensor_scalar(Cb[:, hd, :, :], Cst[:, hd, :, :], dcol, None, ALU.mult), r=[rC[hd], rwtok], w=[rCb[hd]])
            for hd in range(4):
                dcol = wtok[:, 32 + c * 4 + hd:32 + c * 4 + hd + 1]
                bkN, rbkN = nb()
                pe(lambda: PE.matmul(bkN[:, 0:257], PT[:, hd, :], vx[:, c, hd, :], start=True, stop=False), r=[rPT[hd], rvx[c]], w=[rbkN])
                for dc in range(2):
                    pe(lambda: PE.matmul(bkN[:, 0:257], SL[:, Q0 + hd * 2 + dc, cs], Cb[:, hd, dc, :], start=False, stop=(dc == 1)),
                       r=[rsl[Q0 + hd * 2 + dc], rCb[hd]], w=[rbkN])
                act(lambda: A.copy(nd[:, hd, :], bkN[:, 0:257]), r=[rbkN], w=[rnd[hd]])
                ji = ring("junk", 1)
                act(lambda: A.activation(junk[ji][:, 0:256], bkN[:, 0:256], AF.Square, accum_out=fin[:, hd:hd + 1]), r=[rbkN], w=[rjunk[ji], rfin])
                for dc in range(2):
                    bkU, rbkU = nb()
                    pe(lambda: PE.matmul(bkU[:, 0:257], kw[:, hd, dc * 128:(dc + 1) * 128], vx[:, c, hd, :], start=True, stop=True),
                       r=[rkw[hd], rvx[c]], w=[rbkU])
                    dve(lambda: V.scalar_tensor_tensor(Cst[:, hd, dc, :], Cst[:, hd, dc, :], dcol, bkU[:, 0:257], ALU.mult, ALU.add),
                        r=[rC[hd], rwtok, rbkU, rCb[hd]], w=[rC[hd]])
            den = nd[:, :, 256]
            thr = wtok[:, 16 + c * 4:16 + c * 4 + 4]
            dve(lambda: V.scalar_tensor_tensor(fin[:, 4:8], den, -1.0, den, ALU.mult, ALU.max), r=rnd, w=[rfin])
            dve(lambda: V.tensor_tensor(fin[:, 4:8], fin[:, 4:8], thr, ALU.max), r=[rfin, rwtok], w=[rfin])
            dve(lambda: V.reciprocal(fin[:, 4:8], fin[:, 4:8]), r=[rfin], w=[rfin])
            dve(lambda: V.tensor_tensor(fin[:, 8:12], fin[:, 4:8], fin[:, 4:8], ALU.mult), r=[rfin], w=[rfin])
            dve(lambda: V.tensor_tensor(fin[:, 8:12], fin[:, 8:12], fin[:, 0:4], ALU.mult), r=[rfin], w=[rfin])
            dve(lambda: V.tensor_scalar(fin[:, 8:12], fin[:, 8:12], 1.0 / 256.0, EPS, ALU.mult, ALU.add), r=[rfin], w=[rfin])
            act(lambda: A.activation(fin[:, 8:12], fin[:, 8:12], AF.Sqrt), r=[rfin], w=[rfin])
            dve(lambda: V.reciprocal(fin[:, 8:12], fin[:, 8:12]), r=[rfin], w=[rfin])
            dve(lambda: V.tensor_tensor(fin[:, 12:16], fin[:, 4:8], fin[:, 8:12], ALU.mult), r=[rfin], w=[rfin])
            hi = ring("hmn", 1)
            for hd in range(4):
                dve(lambda: V.scalar_tensor_tensor(hmn[hi][:, hd * 256:(hd + 1) * 256], nd[:, hd, 0:256], fin[:, 12 + hd:13 + hd], og[:, c, hd * 256:(hd + 1) * 256], ALU.mult, ALU.mult),
                    r=[rnd[hd], rfin, rog[c]], w=[rhmn[hi]])
            bk2, rbk2 = nb()
            bv2 = bfv(bk2)
            for i in range(8):
                pe(lambda: PE.transpose(bv2[:, i, :], hmn[hi][:, i * 128:(i + 1) * 128], ident), r=[rhmn[hi], rcst], w=[rbk2])
            dve(lambda: V.tensor_tensor(SL[:, SL_CAT:SL_CAT + 8, cs], bv2[:, :, :], g_mls.unsqueeze(2).broadcast_to([128, 8, 128]), ALU.mult),
                r=[rbk2, rcolp], w=rsl[SL_CAT:SL_CAT + 8])

        swa_prev = {"vpad": None}

        def swa_block(t, c):
            cs = slice(c * 128, (c + 1) * 128)
            has_prev = not (t == 0 and c == 0)
            vi = ring("vpad", 3)
            vp, rvp = vpad[vi], rvpad[vi]
            vsrc = vtok[:, c, :].rearrange("p (g d) -> p g d", d=64)
            dve(lambda: V.tensor_copy(vp[:, :, 0, 0:64], vsrc), r=[rvtok[c]], w=[rvp])
            dve(lambda: V.tensor_copy(vp[:, :, 1, 64:128], vsrc), r=[rvtok[c]], w=[rvp])
            pv = swa_prev["vpad"]
            for g in range(2):
                kinds = [(par, pr) for par in range(2) for pr in ((0, 1) if has_prev else (0,))]
                bks = {}
                for (par, pr) in kinds:
                    bk, rbk = nb()
                    bks[(par, pr)] = (bk, rbk)
                    pe(lambda: PE.matmul(bk[:, :], ident, mprev4 if pr else mcur4, start=True, stop=False), r=[rcst], w=[rbk])
                for pair in range(4):
                    qs = SL_QA + g * 4 + pair
                    for (par, pr) in kinds:
                        bk, rbk = bks[(par, pr)]
                        ps_ = slice(par * 64, (par + 1) * 64)
                        if not pr:
                            kap, kres = SL[ps_, SL_KA + g, cs], rsl[SL_KA + g]
                        elif c > 0:
                            kap, kres = SL[ps_, SL_KA + g, (c - 1) * 128:c * 128], rsl[SL_KA + g]
                        else:
                            kap, kres = prevK[ps_, g, :], rprevK
                        pe(lambda: PE.matmul(bk[:, pair * 128:(pair + 1) * 128], kap, SL[ps_, qs, cs], start=False, stop=(pair == 3)),
                           r=[kres, rsl[qs]], w=[rbk])
                pts = {}
                for n_, (par, pr) in enumerate(kinds):
                    bk, rbk = bks[(par, pr)]
                    sl = SL_PT + g * 4 + n_
                    pts[(par, pr)] = sl
                    act(lambda: A.activation(SL[:, sl, :], bk[:, :], AF.Exp, scale=0.125), r=[rbk], w=[rsl[sl]])
                bkD, rbkD = nb()
                bkO, rbkO = nb()
                pe(lambda: PE.matmul(bkD[:, :], Asink[0:8, g, :], cf[0:8, CF_BSEL:CF_BSEL + 512], start=True, stop=False), r=[rAs, rcst], w=[rbkD])
                for n_, (par, pr) in enumerate(kinds):
                    last = (n_ == len(kinds) - 1)
                    sl = pts[(par, pr)]
                    pe(lambda: PE.matmul(bkD[:, :], onesB if par else onesA, SL[:, sl, :], start=False, stop=last), r=[rcst, rsl[sl]], w=[rbkD])
                    vsrc_t, vres = (pv if pr else (vp, rvp))
                    pe(lambda: PE.matmul(bkO[:, :], vsrc_t[:, g, par, :], SL[:, sl, :], start=(n_ == 0), stop=last), r=[vres, rsl[sl]], w=[rbkO])
                ri = ring("rd", 2)
                dve(lambda: V.reciprocal(rd[ri][:], bkD[:, :]), r=[rbkD], w=[rrd[ri]])
                dve(lambda: V.tensor_tensor(SL[:, SL_CAT + 8 + g * 4:SL_CAT + 8 + g * 4 + 4, cs],
                                            bkO[:, :].rearrange("p (a b) -> p a b", b=128),
                                            rd[ri][:].rearrange("p (a b) -> p a b", b=128), ALU.mult),
                    r=[rbkO, rrd[ri]], w=rsl[SL_CAT + 8 + g * 4:SL_CAT + 8 + g * 4 + 4])
            swa_prev["vpad"] = (vp, rvp)

        for s in range(nseq):
            if not nomem:
                mem_stage(s)
            for hd in range(4):
                dve(lambda: V.memset(Cst[:, hd, :, :], 0.0), w=[rC[hd]])
            for t in range(ntile):
                tile(s, t)
        for a_ in out_accs:
            S._wait("sp", a_)
        S.wait_all("sp", rwb)
        stats = dict(inst=S.n_inst, waits=S.n_wait, incs=S.n_inc)
    return nc, dbg_items, stats


_CONSTS = None


def make_in_maps(inputs):
    global _CONSTS
    if _CONSTS is None:
        _CONSTS = _consts()
    cbv, cfv = _CONSTS
    f = lambda a: np.ascontiguousarray(np.asarray(a, dtype=np.float32))
    conv = f(inputs["conv_qk"][0])
    colp = np.concatenate([
        conv.reshape(4 * 16, 128),
        f(inputs["mix_pre_g"][0]).reshape(8, 128),
        f(inputs["xattn_pre_g"][0]).reshape(8, 128),
        f(inputs["ffn_pre_g"][0]).reshape(8, 128),
        f(inputs["mem_norm_g"][0]).reshape(8, 128),
        f(inputs["mlstm_norm_g"][0]).reshape(8, 128),
        np.zeros((24, 128), np.float32),
    ], axis=0)
    rowg = np.stack([f(inputs["mix_post_g"][0]), f(inputs["xattn_post_g"][0]), f(inputs["ffn_post_g"][0])], axis=0)
    gb = np.stack([f(inputs["i_bias"][0]), f(inputs["f_bias"][0])], axis=1)
    sk = np.ascontiguousarray(f(inputs["attn_sinks"][0]).reshape(2, 8).T)
    shared = dict(
        w_in=f(inputs["w_in"][0]), w_out=f(inputs["w_out"][0]), w_xq=f(inputs["w_xq"][0]),
        w_xkv=f(inputs["w_xkv"][0]), w_xo=f(inputs["w_xo"][0]), w_gu=f(inputs["w_gate_up"][0]),
        w_dn=f(inputs["w_down"][0]), colp=np.ascontiguousarray(colp), rowg=np.ascontiguousarray(rowg),
        gb=np.ascontiguousarray(gb), sk=sk, cstb=cbv, cstf=cfv)
    x = f(inputs["x"])
    mem = f(inputs["mem"])
    pos = np.ascontiguousarray(np.asarray(inputs["positions"], dtype=np.int32))
    maps = []
    for i in range(NCORES):
        m = dict(shared)
        m["x"] = x[2 * i:2 * i + 2]
        m["mem"] = mem[2 * i:2 * i + 2]
        m["pos"] = pos[2 * i:2 * i + 2]
        maps.append(m)
    return maps


def kernel(**inputs):
    nc, _, _ = build()
    maps = make_in_maps(inputs)
    res = run_bass_kernel_spmd(nc, maps, core_ids=list(range(NCORES)))
    out = np.concatenate([np.asarray(r["y"]) for r in res.results], axis=0)
    return out.astype(np.float32)
```

```python
import numpy as np
import ml_dtypes
from contextlib import ExitStack
import concourse.bass as bass
import concourse.mybir as mybir
from concourse.bass_utils import run_bass_kernel_spmd

F32 = mybir.dt.float32
BF16 = mybir.dt.bfloat16
I32 = mybir.dt.int32
AF = mybir.ActivationFunctionType
ALU = mybir.AluOpType
AX = mybir.AxisListType

NCORES = 8
SEQ = 2048
D = 1024
T = 512
EPS = 1e-6
IN_W = 5384
DFF = 2816
NEG = -30000.0
LN16 = float(np.log(16.0))
SINSC = float(2 * np.pi * (1 - 1e-6))


class Res:
    __slots__ = ("name", "w", "rs", "excl")

    def __init__(self, name, excl=False):
        self.name = name
        self.w = []
        self.rs = []
        self.excl = excl


class Acc:
    __slots__ = ("eng", "seq", "ev")

    def __init__(self, eng, seq, ev=None):
        self.eng = eng
        self.seq = seq
        self.ev = ev


class Sync:
    ENGS = ("pe", "dve", "act", "pool", "sp")

    def __init__(self, nc, stack, n_dma_sems=24):
        self.nc = nc
        self.eng = {"pe": nc.tensor, "dve": nc.vector, "act": nc.scalar,
                    "pool": nc.gpsimd, "sp": nc.sync}
        self.sem = {}
        self.cnt = {}
        for e in self.ENGS:
            self.sem[e] = stack.enter_context(nc.semaphore("s_" + e))
            self.cnt[e] = 0
        self.dma_sems = {"sp": [], "pool": []}
        self.dma_last = {}
        for q, n in (("sp", n_dma_sems), ("pool", 16)):
            for i in range(n):
                key = "dma_%s%d" % (q, i)
                self.sem[key] = stack.enter_context(nc.semaphore("s_" + key))
                self.cnt[key] = 0
                self.dma_sems[q].append(key)
                self.dma_last[key] = None
        self.dma_rr = {"sp": 0, "pool": 0}
        self.known = {e: {} for e in self.ENGS}
        self.seq = {e: 0 for e in self.ENGS}
        self.last_inst = {e: None for e in self.ENGS}
        self.inc_log = {e: [] for e in self.ENGS}
        self.insts = {e: [] for e in self.ENGS}
        self.kn_hist = {e: [] for e in self.ENGS}
        self.n_wait = 0
        self.n_inc = 0
        self.n_inst = 0

    def _materialise(self, acc):
        if acc.ev is not None:
            return acc.ev
        e = acc.eng
        log = self.inc_log[e]
        lo, hi = 0, len(log)
        while lo < hi:
            mid = (lo + hi) // 2
            if log[mid][0] >= acc.seq:
                hi = mid
            else:
                lo = mid + 1
        if lo < len(log):
            _, val, clock = log[lo]
            acc.ev = (e, val, clock)
            return acc.ev
        seq = acc.seq
        inst = self.insts[e][seq - 1]
        inst.then_inc(self.sem[e], 1)
        self.n_inc += 1
        self.cnt[e] += 1
        hist = self.kn_hist[e]
        lo, hi = 0, len(hist)
        while lo < hi:
            mid = (lo + hi) // 2
            if hist[mid][0] <= seq:
                lo = mid + 1
            else:
                hi = mid
        clock = dict(hist[lo - 1][1]) if lo > 0 else {}
        clock[e] = self.cnt[e]
        log.append((seq, self.cnt[e], clock))
        acc.ev = (e, self.cnt[e], clock)
        return acc.ev

    def _wait(self, e, acc):
        key, val, clock = self._materialise(acc)
        kn = self.known[e]
        if kn.get(key, 0) >= val:
            return
        self.eng[e].wait_ge(self.sem[key], val)
        self.n_wait += 1
        for k, v in clock.items():
            if kn.get(k, 0) < v:
                kn[k] = v
        if kn.get(key, 0) < val:
            kn[key] = val
        if e in self.kn_hist:
            self.kn_hist[e].append((self.seq[e] + 1, dict(kn)))

    STRICT = False

    def _dep(self, e, acc, raw):
        if acc.eng == e:
            if e == "pe" or not (raw or self.STRICT):
                return
        self._wait(e, acc)

    def _deps(self, e, reads, writes):
        for r in reads:
            for a in r.w:
                self._dep(e, a, True)
        for w in writes:
            for a in w.w:
                self._dep(e, a, False if a.eng == e else True)
            for a in w.rs:
                self._dep(e, a, False)

    def op(self, e, fn, reads=(), writes=()):
        ex = [r for r in reads if r.excl]
        if ex:
            reads = [r for r in reads if not r.excl]
            writes = list(writes) + ex
        self._deps(e, reads, writes)
        inst = fn()
        self.n_inst += 1
        self.seq[e] += 1
        s = self.seq[e]
        self.insts[e].append(inst)
        acc = Acc(e, s)
        for r in reads:
            r.rs.append(acc)
        for w in writes:
            w.w = [acc]
            w.rs = []
        return inst

    def dma(self, q, pairs, reads=(), writes=()):
        self._deps(q, reads, writes)
        accs = []
        for (out, in_) in pairs:
            key = self.dma_sems[q][self.dma_rr[q]]
            self.dma_rr[q] = (self.dma_rr[q] + 1) % len(self.dma_sems[q])
            prev = self.dma_last[key]
            if prev is not None:
                self._wait(q, prev)
            inst = self.eng[q].dma_start(out=out, in_=in_)
            inst.then_inc(self.sem[key], 16)
            self.n_inst += 1
            self.cnt[key] += 16
            acc = Acc(key, 0, (key, self.cnt[key], dict(self.known[q])))
            self.dma_last[key] = acc
            accs.append(acc)
        for r in reads:
            r.rs.extend(accs)
        for w in writes:
            w.w = list(accs)
            w.rs = []
        return accs

    def wait_all(self, e, resources):
        for r in resources:
            for a in r.w:
                self._wait(e, a)
            for a in r.rs:
                self._wait(e, a)


CB_IDENT, CB_MASKT, CB_PM, CB_MCUR, CB_MPREV, CB_ONES, CB_ONESA, CB_ONESB, CB_BSEL = 0, 128, 256, 384, 896, 1408, 1536, 1664, 1792
NCB = 1792 + 512
CF_IDENT, CF_INVF, CF_HALF, CF_QUART, CF_SCS, CF_BIS, CF_PAR, CF_EYE4, CF_ONES4, CF_EPS = 0, 128, 129, 130, 131, 132, 133, 261, 265, 393
NCF = 394


def _consts():
    cb = np.zeros((128, NCB), np.float32)
    cb[:, CB_IDENT:CB_IDENT + 128] = np.eye(128)
    i = np.arange(128)[:, None]
    j = np.arange(128)[None, :]
    cb[:, CB_MASKT:CB_MASKT + 128] = (j >= i)
    p = np.arange(128)
    partner = np.where((p % 64) < 32, p + 32, p - 32)
    pm = np.zeros((128, 128), np.float32)
    pm[partner, p] = 1.0
    cb[:, CB_PM:CB_PM + 128] = pm
    mcur = np.where(j >= i, 0.0, NEG)
    mprev = np.where(j < i, 0.0, NEG)
    cb[:, CB_MCUR:CB_MCUR + 512] = np.tile(mcur, (1, 4))
    cb[:, CB_MPREV:CB_MPREV + 512] = np.tile(mprev, (1, 4))
    cb[:, CB_ONES:CB_ONES + 128] = 1.0
    cb[:, CB_ONESA:CB_ONESA + 64] = 1.0
    cb[:, CB_ONESB + 64:CB_ONESB + 128] = 1.0
    cf = np.zeros((128, NCF), np.float32)
    cf[:, CF_IDENT:CF_IDENT + 128] = np.eye(128)
    inv = 10000.0 ** (-np.arange(0, 64, 2, dtype=np.float32) / 64.0)
    d = (p % 64) % 32
    cf[:, CF_INVF] = inv[d] / (2 * np.pi)
    cf[:, CF_HALF] = 0.5
    cf[:, CF_QUART] = 0.25
    sgn = np.where((p % 64) < 32, -1.0, 1.0)
    cf[:, CF_SCS] = sgn * SINSC
    cf[:, CF_BIS] = -sgn * SINSC / 2
    k8 = np.arange(8)[:, None]
    cf[0:8, CF_PAR:CF_PAR + 128] = ((p[None, :] >= 64) == (k8 % 2 == 1))
    cf[0:4, CF_EYE4:CF_EYE4 + 4] = np.eye(4)
    cf[0:4, CF_ONES4:CF_ONES4 + 128] = 1.0
    col = np.arange(512)[None, :]
    cf[:, CF_EPS] = EPS
    cb[0:8, CB_BSEL:CB_BSEL + 512] = ((col // 128) == (k8 // 2))
    cb[32:40, CB_BSEL:CB_BSEL + 512] = ((col // 128) == (k8 // 2))
    cf[32:40, CF_PAR:CF_PAR + 128] = cf[0:8, CF_PAR:CF_PAR + 128]
    return cb.astype(ml_dtypes.bfloat16), cf.astype(np.float32)


SL_U = 0
SL_QK = 8
SL_QA = 24
SL_KA = 32
SL_CAT = 34
SL_PT = 50
NSLAB = 58
NWB = 3
NFT = 7


def build(nseq=2, ntile=4, dbg=None, stop=None, nomem=False):
    nc = bass.Bass("TRN2", target_bir_lowering=False)
    dt = lambda n, s, d, k="ExternalInput": nc.dram_tensor(n, list(s), d, kind=k).ap()
    x_d = dt("x", [2, SEQ, D], F32)
    mem_d = dt("mem", [2, 256, D], F32)
    pos_d = dt("pos", [2, SEQ], I32)
    w_in_d = dt("w_in", [D, IN_W], F32)
    w_out_d = dt("w_out", [2 * D, D], F32)
    w_xq_d = dt("w_xq", [D, D], F32)
    w_xkv_d = dt("w_xkv", [D, 2 * D], F32)
    w_xo_d = dt("w_xo", [D, D], F32)
    w_gu_d = dt("w_gu", [D, 2 * DFF], F32)
    w_dn_d = dt("w_dn", [DFF, D], F32)
    colp_d = dt("colp", [128, 128], F32)
    rowg_d = dt("rowg", [3, D], F32)
    gb_d = dt("gb", [4, 2], F32)
    sk_d = dt("sk", [8, 2], F32)
    cstb_d = dt("cstb", [128, NCB], BF16)
    cstf_d = dt("cstf", [128, NCF], F32)
    y_d = dt("y", [2, SEQ, D], F32, "ExternalOutput")
    dbg_items = []
    if dbg:
        dbgf_d = dt("dbgf", [128, 32768], F32, "ExternalOutput")
        dbgb_d = dt("dbgb", [128, 65536], BF16, "ExternalOutput")
    dbg_off = {"f": 0, "b": 0}
    out_accs = []

    w_in_v = w_in_d.rearrange("(k p) n -> p k n", p=128)
    w_out_v = w_out_d.rearrange("(k p) n -> p k n", p=128)
    w_xq_v = w_xq_d.rearrange("(k p) n -> p k n", p=128)
    w_xkv_v = w_xkv_d.rearrange("(k p) n -> p k n", p=128)
    w_xo_v = w_xo_d.rearrange("(k p) n -> p k n", p=128)
    w_gu_v = w_gu_d.rearrange("(k p) n -> p k n", p=128)
    w_dn_v = w_dn_d.rearrange("(k p) n -> p k n", p=128)

    with ExitStack() as st:
        S = Sync(nc, st)
        V, A, PE = nc.vector, nc.scalar, nc.tensor
        sb = lambda n, s, d: st.enter_context(nc.sbuf_tensor("sb_" + n, list(s), d))
        dve = lambda fn, r=(), w=(): S.op("dve", fn, r, w)
        act = lambda fn, r=(), w=(): S.op("act", fn, r, w)
        pe = lambda fn, r=(), w=(): S.op("pe", fn, r, w)

        cb = sb("cstb", [128, NCB], BF16)
        cf = sb("cstf", [128, NCF], F32)
        rcst = Res("cst")
        ident = cb[:, CB_IDENT:CB_IDENT + 128]
        maskT = cb[:, CB_MASKT:CB_MASKT + 128]
        Pm = cb[:, CB_PM:CB_PM + 128]
        mcur4 = cb[:, CB_MCUR:CB_MCUR + 512]
        mprev4 = cb[:, CB_MPREV:CB_MPREV + 512]
        ones_bf = cb[:, CB_ONES:CB_ONES + 128]
        onesA = cb[:, CB_ONESA:CB_ONESA + 128]
        onesB = cb[:, CB_ONESB:CB_ONESB + 128]
        identf = cf[:, CF_IDENT:CF_IDENT + 128]

        banks = [st.enter_context(nc.psum_tensor("bk%d" % i, [128, 512], F32)) for i in range(8)]
        rbank = [Res("bk%d" % i, excl=True) for i in range(8)]
        bank_rr = [0]

        bank_mode = {"split": False}
        bank_rr2 = {"swa": 0, "oth": 0}

        def nb(kind="oth"):
            if bank_mode["split"]:
                j = bank_rr2[kind]
                bank_rr2[kind] = (j + 1) % 4
                i = j if kind == "swa" else 4 + j
                return banks[i], rbank[i]
            i = bank_rr[0]
            bank_rr[0] = (i + 1) % 8
            return banks[i], rbank[i]

        def bfv(bk):
            return bk[:].bitcast(BF16).rearrange("p (a b) -> p a b", b=128)

        SL = sb("SL", [128, NSLAB, 512], BF16)
        rsl = [Res("sl%d" % i) for i in range(NSLAB)]
        h = sb("h", [128, 4, D], F32)
        rh = [Res("h%d" % c) for c in range(4)]
        TOK = sb("tok", [128, 8256], BF16)
        og = TOK[:, 0:4096].rearrange("p (c d) -> p c d", d=1024)
        vx = TOK[:, 4096:4096 + 4112].rearrange("p (c h e) -> p c h e", h=4, e=257)
        abuf = TOK[:, 0:8192].bitcast(F32).rearrange("p (c d) -> p c d", d=1024)
        rog = [Res("og%d" % c) for c in range(4)]
        rvx = [Res("vx%d" % c) for c in range(4)]
        rab = [[rog[0], rog[1]], [rog[2], rog[3]], [rvx[0], rvx[1]], [rvx[1], rvx[2], rvx[3]]]
        wbuf = [sb("wb%d" % i, [128, 8, 512], BF16) for i in range(NWB)]
        rwb = [Res("wb%d" % i) for i in range(NWB)]
        ft = [sb("ft%d" % i, [128, 520], F32) for i in range(NFT)]
        rft = [Res("ft%d" % i) for i in range(NFT)]
        ft_rr = [0]

        def nft():
            i = ft_rr[0]
            ft_rr[0] = (i + 1) % NFT
            return ft[i], rft[i]

        utok = [sb("utok%d" % i, [128, D], BF16) for i in range(2)]
        rutok = [Res("utok%d" % i) for i in range(2)]
        junk = [sb("junk%d" % i, [128, D], BF16) for i in range(1)]
        rjunk = [Res("junk%d" % i) for i in range(1)]
        rr2 = {"utok": 0, "junk": 0, "qb": 0, "hmn": 0, "rd": 0, "vpad": 0}

        def ring(name, n):
            i = rr2[name]
            rr2[name] = (i + 1) % n
            return i

        cosT = sb("cosT", [128, 512], F32)
        sinS = sb("sinS", [128, 512], F32)
        rcos, rsin = Res("cos"), Res("sin")
        qb = [sb("qb%d" % i, [128, 512], BF16) for i in range(2)]
        rqb = [Res("qb%d" % i) for i in range(2)]
        GX = [sb("gx%d" % i, [4, 512], F32) for i in range(3)]
        rgx = [Res("gx%d" % i) for i in range(3)]
        gsm = sb("gsm", [4, 64], F32)
        rgsm = Res("gsm")
        mall = sb("mall", [4, 8], F32)
        rmall = Res("mall")
        Rm = sb("Rm", [4, 16], F32)
        rRm = Res("Rm")
        wtok = sb("wtok", [128, 48], F32)
        rwtok = Res("wtok")
        Cst = sb("Cst", [128, 4, 2, 257], F32)
        rC = [Res("C%d" % i) for i in range(4)]
        Cb = sb("Cb", [128, 4, 2, 257], BF16)
        rCb = [Res("Cb%d" % i) for i in range(4)]
        PT = sb("PT", [128, 4, 128], BF16)
        rPT = [Res("PT%d" % i) for i in range(4)]
        kw = sb("kw", [128, 4, 256], BF16)
        rkw = [Res("kw%d" % i) for i in range(4)]
        nd = sb("nd", [128, 4, 257], F32)
        rnd = [Res("nd%d" % i) for i in range(4)]
        fin = sb("fin", [128, 32], F32)
        rfin = Res("fin")
        hmn = [sb("hmn%d" % i, [128, D], BF16) for i in range(1)]
        rhmn = [Res("hmn%d" % i) for i in range(1)]
        tails = sb("tails", [128, 16, 3], F32)
        rtails = [Res("tail%d" % i) for i in range(16)]
        vtok = sb("vtok", [128, 4, 128], BF16)
        rvtok = [Res("vtok%d" % c) for c in range(4)]
        vpad = [sb("vpad%d" % i, [128, 2, 2, 128], BF16) for i in range(3)]
        rvpad = [Res("vpad%d" % i) for i in range(3)]
        prevK = sb("prevK", [128, 2, 128], BF16)
        rprevK = Res("prevK")
        rd = [sb("rd%d" % i, [128, 512], F32) for i in range(2)]
        rrd = [Res("rd%d" % i) for i in range(2)]
        KmT = sb("KmT", [128, 8, 256], BF16)
        rKm = Res("KmT")
        Vm = sb("Vm", [128, 2, D], BF16)
        rVm = Res("Vm")
        memT = SL[:, SL_PT:SL_PT + 4, :].rearrange("p a (b c) -> p (a b) c", c=256)
        rmemT = None
        grow = sb("grow", [128, 3, D], F32)
        rgrow = Res("grow")
        colst = sb("colst", [128, 128], F32)
        colp = sb("colp", [128, 128], F32)
        rcolst, rcolp = Res("colst"), Res("colp")
        gbt = sb("gbt", [4, 4], F32)
        rgbt = Res("gbt")
        skt = sb("skt", [40, 4], F32)
        rskt = Res("skt")
        Asink = sb("Asink", [40, 2, 128], F32)
        A40 = sb("A40", [40, 2, 128], BF16)
        Alo = sb("Alo", [40, 2, 128], F32)
        rAs = Res("Asink")
        nsm = sb("nsm", [128, 16], F32)
        rnsm = Res("nsm")
        ssp = sb("ssp", [128, 8], F32)
        rssp = [Res("ssp%d" % c) for c in range(4)]
        n2 = sb("n2", [128, 12], F32)
        rn2 = [Res("n2_%d" % c) for c in range(4)]
        ry = Res("y")
        rdbg = Res("dbg")

        convw = lambda j, o: colp[:, j * 16 + o: j * 16 + o + 1]
        g_pre1 = colp[:, 64:72]
        g_xpre = colp[:, 72:80]
        g_fpre = colp[:, 80:88]
        g_mem = colp[:, 88:96]
        g_mls = colp[:, 96:104]

        def dump(name, ap, res, kind):
            if not dbg or name not in dbg:
                return
            shp = list(ap.shape)
            n = int(np.prod(shp[1:]))
            o = dbg_off[kind]
            dbg_off[kind] += n
            dst = (dbgf_d if kind == "f" else dbgb_d)[0:shp[0], o:o + n]
            if len(shp) == 3:
                dst = dst.rearrange("p (a b) -> p a b", b=shp[2])
            elif len(shp) == 4:
                dst = dst.rearrange("p (a b c) -> p a b c", b=shp[2], c=shp[3])
            out_accs.extend(S.dma("sp", [(dst, ap)], reads=list(res), writes=[rdbg]))
            dbg_items.append((name, kind, o, shp))

        S.dma("sp", [(cb[:], cstb_d[:, :]), (cf[:], cstf_d[:, :])], writes=[rcst])
        S.dma("sp", [(colst[:], colp_d[:, :])], writes=[rcolst])
        S.dma("sp", [(grow[:, i, :], rowg_d[i:i + 1, :].partition_broadcast(128)) for i in range(3)], writes=[rgrow])
        S.dma("sp", [(gbt[0:4, 0:2], gb_d[:, :])], writes=[rgbt])
        dve(lambda: V.memset(skt[:, :], 0.0), w=[rskt])
        S.dma("sp", [(skt[0:8, 0:2], sk_d[:, :]), (skt[32:40, 0:2], sk_d[:, :])], writes=[rskt])
        bk, rbk = nb()
        pe(lambda: PE.transpose(bk[:, 0:128], colst[:, :], identf), r=[rcolst, rcst], w=[rbk])
        dve(lambda: V.tensor_copy(colp[:], bk[:, 0:128]), r=[rbk], w=[rcolp])
        dve(lambda: V.tensor_scalar(gbt[0:4, 2:3], gbt[0:4, 1:2], -1.0, None, ALU.mult), r=[rgbt], w=[rgbt])
        act(lambda: A.activation(skt[0:40, 2:4], skt[0:40, 0:2], AF.Exp), r=[rskt], w=[rskt])
        for g in range(2):
            dve(lambda: V.tensor_scalar(Asink[0:40, g, :], cf[0:40, CF_PAR:CF_PAR + 128], skt[0:40, 2 + g:3 + g], None, ALU.mult),
                r=[rskt, rcst], w=[rAs])
        dve(lambda: V.tensor_copy(A40[0:40, :, :], Asink[0:40, :, :]), r=[rAs], w=[rAs])
        dve(lambda: V.tensor_tensor(Alo[0:40, :, :], Asink[0:40, :, :], A40[0:40, :, :], ALU.subtract), r=[rAs], w=[rAs])
        dve(lambda: V.tensor_copy(A40[32:40, :, :], Alo[32:40, :, :]), r=[rAs], w=[rAs])
        for i in range(3):
            dve(lambda: V.memset(vpad[i][:], 0.0), w=[rvpad[i]])

        def seg(view, k0, nk, c0, ncol, dcol=0):
            return (lambda wb: wb[:, 0:nk, dcol:dcol + ncol], view[:, k0:k0 + nk, c0:c0 + ncol])

        specs = []
        for s in range(nseq):
            for i in range(4):
                specs.append([seg(w_xkv_v, 0, 8, i * 512, 512)])
            for t in range(ntile):
                for i in range(4):
                    specs.append([seg(w_in_v, 0, 8, i * 512, 512)])
                blk8 = [seg(w_in_v, 0, 8, 4096, 8, 0), seg(w_in_v, 0, 8, 5256, 128, 8)]
                for g in range(2):
                    for r_ in range(2):
                        blk8.append(seg(w_in_v, 0, 8, 5128 + g * 64, 64, 136 + g * 128 + r_ * 64))
                specs.append(blk8)
                specs.append([seg(w_in_v, 0, 8, 4104, 512)])
                specs.append([seg(w_in_v, 0, 8, 4616, 512)])
                for i in range(4, 8):
                    specs.append([seg(w_in_v, 0, 8, i * 512, 512)])
                for half in range(2):
                    for kh in range(2):
                        specs.append([seg(w_out_v, kh * 8, 8, half * 512, 512)])
                for half in range(2):
                    specs.append([seg(w_xq_v, 0, 8, half * 512, 512)])
                for half in range(2):
                    specs.append([seg(w_xo_v, 0, 8, half * 512, 512)])
                for i in range(6):
                    ncol = 512 if i < 5 else 256
                    specs.append([seg(w_gu_v, 0, 8, i * 512, ncol)])
                    specs.append([seg(w_gu_v, 0, 8, DFF + i * 512, ncol)])
                for half in range(2):
                    for (k0, nk) in ((0, 8), (8, 8), (16, 6)):
                        specs.append([seg(w_dn_v, k0, nk, half * 512, 512)])
        wst = {"issued": 0, "taken": 0}

        def getw():
            n = wst["taken"]
            lim = min(len(specs), n + NWB)
            while wst["issued"] < lim:
                m = wst["issued"]
                i = m % NWB
                S.dma("pool", [(f(wbuf[i]), src) for (f, src) in specs[m]], writes=[rwb[i]])
                wst["issued"] += 1
            wst["taken"] += 1
            return wbuf[n % NWB], rwb[n % NWB]

        def rstd_from_ss(n):
            act(lambda: A.activation(nsm[:, 4:4 + n], nsm[:, 0:n], AF.Ln, scale=1.0 / D, bias=cf[:, CF_EPS:CF_EPS + 1]), r=[rnsm, rcst], w=[rnsm])
            act(lambda: A.activation(nsm[:, 8:8 + n], nsm[:, 4:4 + n], AF.Exp, scale=-0.5), r=[rnsm], w=[rnsm])

        def prenorm(srcs, gcol, dsts):
            n = len(srcs)
            for c, (ap, rs) in enumerate(srcs):
                ji = ring("junk", 1)
                act(lambda: A.activation(junk[ji][:], ap, AF.Square, accum_out=nsm[:, c:c + 1]), r=rs, w=[rjunk[ji], rnsm])
            rstd_from_ss(n)
            for c, (ap, rs) in enumerate(srcs):
                ui = ring("utok", 2)
                act(lambda: A.activation(utok[ui][:], ap, AF.Copy, scale=nsm[:, 8 + c:9 + c]), r=rs + [rnsm], w=[rutok[ui]])
                bk, rbk = nb()
                bv = bfv(bk)
                for k in range(8):
                    pe(lambda: PE.transpose(bv[:, k, :], utok[ui][:, k * 128:(k + 1) * 128], ident), r=[rutok[ui], rcst], w=[rbk])
                dap, drs = dsts[c]
                dve(lambda: V.tensor_tensor(dap, bv[:, :, :], gcol.unsqueeze(2).broadcast_to([128, 8, 128]), ALU.mult),
                    r=[rbk, rcolp], w=drs)

        def tok_gemm(kslabs, nblk_per_half, nks):
            for half in range(2):
                bks = [nb() for _ in range(4)]
                ki = 0
                for bi in range(nblk_per_half):
                    wb, rw = getw()
                    nk = nks[bi]
                    for c in range(4):
                        for kk in range(nk):
                            first = (bi == 0 and kk == 0)
                            last = (bi == nblk_per_half - 1 and kk == nk - 1)
                            sl = kslabs[ki + kk]
                            pe(lambda: PE.matmul(bks[c][0][:, :], SL[:, sl, c * 128:(c + 1) * 128], wb[:, kk, 0:512], start=first, stop=last),
                               r=[rsl[sl], rw], w=[bks[c][1]])
                    ki += nk
                for c in range(4):
                    if half == 0:
                        act(lambda: A.copy(abuf[:, c, half * 512:(half + 1) * 512], bks[c][0][:, :]), r=[bks[c][1]], w=rab[c])
                    else:
                        dve(lambda: V.tensor_copy(abuf[:, c, half * 512:(half + 1) * 512], bks[c][0][:, :]), r=[bks[c][1]], w=rab[c])
                    ji = ring("junk", 1)
                    act(lambda: A.activation(junk[ji][:, 0:512], bks[c][0][:, :], AF.Square, accum_out=ssp[:, 2 * c + half:2 * c + half + 1]),
                        r=[bks[c][1]], w=[rjunk[ji], rssp[c]])

        def pre_front(c, src_ap, src_res):
            ji = ring("junk", 1)
            act(lambda: A.activation(junk[ji][:], src_ap, AF.Square, accum_out=n2[:, c:c + 1]), r=src_res, w=[rjunk[ji], rn2[c]])
            act(lambda: A.activation(n2[:, 4 + c:5 + c], n2[:, c:c + 1], AF.Ln, scale=1.0 / D, bias=cf[:, CF_EPS:CF_EPS + 1]), r=[rn2[c], rcst], w=[rn2[c]])
            act(lambda: A.activation(n2[:, 8 + c:9 + c], n2[:, 4 + c:5 + c], AF.Exp, scale=-0.5), r=[rn2[c]], w=[rn2[c]])
            ui = ring("utok", 2)
            act(lambda: A.activation(utok[ui][:], src_ap, AF.Copy, scale=n2[:, 8 + c:9 + c]), r=src_res + [rn2[c]], w=[rutok[ui]])
            bk, rbk = nb()
            bv = bfv(bk)
            for k in range(8):
                pe(lambda: PE.transpose(bv[:, k, :], utok[ui][:, k * 128:(k + 1) * 128], ident), r=[rutok[ui], rcst], w=[rbk])
            return bv, rbk

        def pre_evac(c, bv, rbk, gcol):
            dve(lambda: V.tensor_tensor(SL[:, SL_U:SL_U + 8, c * 128:(c + 1) * 128], bv[:, :, :], gcol.unsqueeze(2).broadcast_to([128, 8, 128]), ALU.mult),
                r=[rbk, rcolp], w=rsl[SL_U:SL_U + 8])

        def norm_boundary(gi, gcol_next, on_done=None):
            dve(lambda: V.tensor_tensor(nsm[:, 0:4], ssp[:, 0:8:2], ssp[:, 1:8:2], ALU.add), r=rssp, w=[rnsm])
            rstd_from_ss(4)
            pend = None
            for c in range(4):
                for hf in range(2):
                    t1, rt1 = nft()
                    sl_ = slice(hf * 512, (hf + 1) * 512)
                    dve(lambda: V.scalar_tensor_tensor(t1[:, 0:512], abuf[:, c, sl_], nsm[:, 8 + c:9 + c], grow[:, gi, sl_], ALU.mult, ALU.mult),
                        r=rab[c] + [rnsm, rgrow], w=[rt1])
                    dve(lambda: V.tensor_tensor(h[:, c, sl_], h[:, c, sl_], t1[:, 0:512], ALU.add), r=[rh[c], rt1], w=[rh[c]])
                if pend is not None:
                    pre_evac(*pend, gcol_next)
                    pend = None
                if gcol_next is not None:
                    bv, rbk = pre_front(c, h[:, c, :], [rh[c]])
                    pend = (c, bv, rbk)
                if on_done is not None:
                    on_done(c)
            if pend is not None:
                pre_evac(*pend, gcol_next)

        def postnorm(gi):
            for c in range(4):
                ji = ring("junk", 1)
                act(lambda: A.activation(junk[ji][:], abuf[:, c, :], AF.Square, accum_out=nsm[:, c:c + 1]), r=rab[c], w=[rjunk[ji], rnsm])
            rstd_from_ss(4)
            for c in range(4):
                for hf in range(2):
                    t1, rt1 = nft()
                    sl_ = slice(hf * 512, (hf + 1) * 512)
                    dve(lambda: V.scalar_tensor_tensor(t1[:, 0:512], abuf[:, c, sl_], nsm[:, 8 + c:9 + c], grow[:, gi, sl_], ALU.mult, ALU.mult),
                        r=rab[c] + [rnsm, rgrow], w=[rt1])
                    dve(lambda: V.tensor_tensor(h[:, c, sl_], h[:, c, sl_], t1[:, 0:512], ALU.add), r=[rh[c], rt1], w=[rh[c]])

        rmemTs = rsl[SL_PT:SL_PT + 4]

        def mem_stage(s):
            S.dma("sp", [(h[:, 0:2, :], mem_d[s].rearrange("(c p) d -> p c d", p=128))], writes=[rh[0], rh[1]])
            prenorm([(h[:, c, :], [rh[c]]) for c in range(2)], g_mem,
                    [(memT[:, :, c * 128:(c + 1) * 128], rmemTs) for c in range(2)])
            for b_ in range(2):
                wb, rw = getw()
                for j in range(4):
                    oc = b_ * 4 + j
                    bk, rbk = nb()
                    for k in range(8):
                        pe(lambda: PE.matmul(bk[:, 0:256], wb[:, k, j * 128:(j + 1) * 128], memT[:, k, :], start=(k == 0), stop=(k == 7)),
                           r=[rw] + rmemTs, w=[rbk])
                    act(lambda: A.copy(KmT[:, oc, :], bk[:, 0:256]), r=[rbk], w=[rKm])
            for b_ in range(2):
                wb, rw = getw()
                for mc in range(2):
                    bk, rbk = nb()
                    for k in range(8):
                        pe(lambda: PE.matmul(bk[:, :], memT[:, k, mc * 128:(mc + 1) * 128], wb[:, k, 0:512], start=(k == 0), stop=(k == 7)),
                           r=[rw] + rmemTs, w=[rbk])
                    act(lambda: A.copy(Vm[:, mc, b_ * 512:(b_ + 1) * 512], bk[:, :]), r=[rbk], w=[rVm])

        def rope_tables(s, t0):
            pi_, rpi = nft()
            yv, ryv = nft()
            fr, rfr = nft()
            posi = pi_[:, 0:512].bitcast(I32)
            S.dma("sp", [(posi, pos_d[s:s + 1, t0:t0 + 512].partition_broadcast(128))], writes=[rpi])
            dve(lambda: V.tensor_copy(yv[:, 0:512], posi), r=[rpi], w=[ryv])
            dve(lambda: V.tensor_scalar(yv[:, 0:512], yv[:, 0:512], cf[:, CF_INVF:CF_INVF + 1], cf[:, CF_HALF:CF_HALF + 1], ALU.mult, ALU.add),
                r=[ryv, rcst], w=[ryv])
            for which in range(2):
                if which == 1:
                    dve(lambda: V.tensor_scalar(yv[:, 0:512], yv[:, 0:512], cf[:, CF_QUART:CF_QUART + 1], None, ALU.add), r=[ryv, rcst], w=[ryv])
                dve(lambda: V.tensor_copy(posi, yv[:, 0:512]), r=[ryv], w=[rpi])
                dve(lambda: V.tensor_tensor(fr[:, 0:512], yv[:, 0:512], posi, ALU.subtract), r=[ryv, rpi], w=[rfr])
                dve(lambda: V.scalar_tensor_tensor(fr[:, 0:512], fr[:, 0:512], 0.0, fr[:, 0:512], ALU.is_lt, ALU.add), r=[rfr], w=[rfr])
                if which == 0:
                    act(lambda: A.activation(sinS[:], fr[:, 0:512], AF.Sin, scale=cf[:, CF_SCS:CF_SCS + 1], bias=cf[:, CF_BIS:CF_BIS + 1]),
                        r=[rfr, rcst], w=[rsin])
                else:
                    act(lambda: A.activation(cosT[:], fr[:, 0:512], AF.Sin, scale=SINSC, bias=-SINSC / 2), r=[rfr], w=[rcos])

        def rope(bk, rbk, dst):
            if stop == "B3d":
                qi = ring("qb", 2)
                act(lambda: A.copy(qb[qi][:], bk[:, :]), r=[rbk], w=[rqb[qi]])
                b2, rb2 = nb()
                pe(lambda: PE.matmul(b2[:, :], Pm, qb[qi][:], start=True, stop=True), r=[rcst, rqb[qi]], w=[rb2])
                act(lambda: A.copy(SL[:, dst, :], b2[:, :]), r=[rb2], w=[rsl[dst]])
                return
            if stop == "B3e":
                t1, rt1 = nft()
                dve(lambda: V.tensor_tensor(t1[:, 0:512], bk[:, :], cosT[:], ALU.mult), r=[rbk, rcos], w=[rt1])
                dve(lambda: V.tensor_tensor(SL[:, dst, :], t1[:, 0:512], sinS[:], ALU.add), r=[rt1, rsin], w=[rsl[dst]])
                return
            qi = ring("qb", 2)
            act(lambda: A.copy(qb[qi][:], bk[:, :]), r=[rbk], w=[rqb[qi]])
            b2, rb2 = nb()
            pe(lambda: PE.matmul(b2[:, :], Pm, qb[qi][:], start=True, stop=True), r=[rcst, rqb[qi]], w=[rb2])
            t1, rt1 = nft()
            t2, rt2 = nft()
            if stop == "B3g":
                dve(lambda: V.tensor_tensor(t1[:, 0:512], bk[:, :], cosT[:], ALU.mult), r=[rbk, rcos], w=[rt1])
                dve(lambda: V.tensor_tensor(t2[:, 0:512], b2[:, :], sinS[:], ALU.mult), r=[rb2, rsin], w=[rt2])
                dve(lambda: V.tensor_copy(SL[:, dst, :], t1[:, 0:512]), r=[rt1, rt2], w=[rsl[dst]])
                return
            if stop == "B3h":
                dve(lambda: V.tensor_tensor(t2[:, 0:512], b2[:, :], sinS[:], ALU.mult), r=[rb2, rsin], w=[rt2])
                dve(lambda: V.tensor_tensor(SL[:, dst, :], t2[:, 0:512], cosT[:], ALU.add), r=[rt2, rcos], w=[rsl[dst]])
                return
            dve(lambda: V.tensor_tensor(t1[:, 0:512], bk[:, :], cosT[:], ALU.mult), r=[rbk, rcos], w=[rt1])
            dve(lambda: V.tensor_tensor(t2[:, 0:512], b2[:, :], sinS[:], ALU.mult), r=[rb2, rsin], w=[rt2])
            dve(lambda: V.tensor_tensor(SL[:, dst, :], t1[:, 0:512], t2[:, 0:512], ALU.add), r=[rt1, rt2], w=[rsl[dst]])

        def tile(s, t):
            t0 = t * T
            first_tile = (t == 0)
            for c in range(4):
                S.dma("sp", [(h[:, c, :], x_d[s, t0 + c * 128:t0 + (c + 1) * 128, :])], writes=[rh[c]])

            def fin_tile():
                out_accs.extend(S.dma("sp", [(y_d[s, t0:t0 + T, :].rearrange("(c p) d -> p c d", p=128), h[:, :, :])], reads=rh, writes=[ry]))
            if stop == "L":
                return fin_tile()
            rope_tables(s, t0)
            if stop == "R":
                return fin_tile()
            pend = None
            for c in range(4):
                bv, rbk = pre_front(c, h[:, c, :], [rh[c]])
                if pend is not None:
                    pre_evac(*pend, g_pre1)
                pend = (c, bv, rbk)
            pre_evac(*pend, g_pre1)
            dump("uT", SL[:, SL_U:SL_U + 8, :], rsl[SL_U:SL_U + 8], "b")
            if stop == "A":
                return fin_tile()

            pend = None
            for b_ in range(4):
                wb, rw = getw()
                for j in range(4):
                    oc = b_ * 4 + j
                    bk, rbk = nb()
                    for k in range(8):
                        pe(lambda: PE.matmul(bk[:, :], wb[:, k, j * 128:(j + 1) * 128], SL[:, SL_U + k, :], start=(k == 0), stop=(k == 7)),
                           r=[rw, rsl[SL_U + k]], w=[rbk])
                    cbf, rcb = nft()
                    acc, racc = nft()
                    act(lambda: A.copy(cbf[:, 3:515], bk[:, :]), r=[rbk], w=[rcb])
                    if first_tile:
                        dve(lambda: V.memset(cbf[:, 0:3], 0.0), w=[rcb])
                    else:
                        dve(lambda: V.tensor_copy(cbf[:, 0:3], tails[:, oc, :]), r=[rtails[oc]], w=[rcb])
                    act(lambda: A.activation(acc[:, 0:512], bk[:, :], AF.Copy, scale=convw(3, oc)), r=[rbk, rcolp], w=[racc])
                    dve(lambda: V.tensor_copy(tails[:, oc, :], cbf[:, 512:515]), r=[rcb], w=[rtails[oc]])
                    for jj in range(3):
                        dve(lambda: V.scalar_tensor_tensor(acc[:, 0:512], cbf[:, jj:jj + 512], convw(jj, oc), acc[:, 0:512], ALU.mult, ALU.add),
                            r=[rcb, racc, rcolp], w=[racc])
                    if pend is not None:
                        pacc, pracc, poc = pend
                        act(lambda: A.activation(SL[:, SL_QK + poc, :], pacc[:, 0:512], AF.Silu), r=[pracc], w=[rsl[SL_QK + poc]])
                    pend = (acc, racc, oc)
            pacc, pracc, poc = pend
            act(lambda: A.activation(SL[:, SL_QK + poc, :], pacc[:, 0:512], AF.Silu), r=[pracc], w=[rsl[SL_QK + poc]])
            dump("qkT", SL[:, SL_QK:SL_QK + 16, :], rsl[SL_QK:SL_QK + 16], "b")
            if stop == "B1":
                return fin_tile()
            wb, rw = getw()
            bkI, rbkI = nb()
            bkF, rbkF = nb()
            for k in range(8):
                pe(lambda: PE.matmul(bkI[0:4, :], wb[:, k, 0:4], SL[:, SL_U + k, :], start=(k == 0), stop=(k == 7)), r=[rw, rsl[SL_U + k]], w=[rbkI])
            for k in range(8):
                pe(lambda: PE.matmul(bkF[0:4, :], wb[:, k, 4:8], SL[:, SL_U + k, :], start=(k == 0), stop=(k == 7)), r=[rw, rsl[SL_U + k]], w=[rbkF])
            gates(bkI, rbkI, bkF, rbkF, first_tile)
            if stop == "B3":
                return fin_tile()
            for c in range(4):
                bk, rbk = nb()
                for k in range(8):
                    pe(lambda: PE.matmul(bk[:, 0:128], SL[:, SL_U + k, c * 128:(c + 1) * 128], wb[:, k, 8:136], start=(k == 0), stop=(k == 7)),
                       r=[rw, rsl[SL_U + k]], w=[rbk])
                act(lambda: A.copy(vtok[:, c, :], bk[:, 0:128]), r=[rbk], w=[rvtok[c]])
            if stop == "B3a":
                return fin_tile()
            if not first_tile:
                for g in range(2):
                    dve(lambda: V.tensor_copy(prevK[:, g, :], SL[:, SL_KA + g, 384:512]), r=[rsl[SL_KA + g]], w=[rprevK])
            for g in range(2):
                bk, rbk = nb()
                for k in range(8):
                    pe(lambda: PE.matmul(bk[:, :], wb[:, k, 136 + g * 128:136 + (g + 1) * 128], SL[:, SL_U + k, :], start=(k == 0), stop=(k == 7)),
                       r=[rw, rsl[SL_U + k]], w=[rbk])
                if stop == "B3c":
                    act(lambda: A.copy(SL[:, SL_KA + g, :], bk[:, :]), r=[rbk], w=[rsl[SL_KA + g]])
                else:
                    rope(bk, rbk, SL_KA + g)
            if stop in ("B3b", "B3c", "B3d", "B3e", "B3g", "B3h"):
                return fin_tile()
            for b_ in range(2):
                wb, rw = getw()
                for j in range(4):
                    bk, rbk = nb()
                    for k in range(8):
                        pe(lambda: PE.matmul(bk[:, :], wb[:, k, j * 128:(j + 1) * 128], SL[:, SL_U + k, :], start=(k == 0), stop=(k == 7)),
                           r=[rw, rsl[SL_U + k]], w=[rbk])
                    rope(bk, rbk, SL_QA + b_ * 4 + j)
            for c in range(4):
                dve(lambda: V.memset(vx[:, c, :, 256:257], 1.0), w=[rvx[c]])
            for b_ in range(2):
                wb, rw = getw()
                for c in range(4):
                    bk, rbk = nb()
                    for k in range(8):
                        pe(lambda: PE.matmul(bk[:, :], SL[:, SL_U + k, c * 128:(c + 1) * 128], wb[:, k, 0:512], start=(k == 0), stop=(k == 7)),
                           r=[rw, rsl[SL_U + k]], w=[rbk])
                    act(lambda: A.copy(vx[:, c, 2 * b_:2 * b_ + 2, 0:256], bk[:, :].rearrange("p (h e) -> p h e", e=256)), r=[rbk], w=[rvx[c]])
            for b_ in range(2):
                wb, rw = getw()
                for c in range(4):
                    bk, rbk = nb()
                    for k in range(8):
                        pe(lambda: PE.matmul(bk[:, :], SL[:, SL_U + k, c * 128:(c + 1) * 128], wb[:, k, 0:512], start=(k == 0), stop=(k == 7)),
                           r=[rw, rsl[SL_U + k]], w=[rbk])
                    act(lambda: A.activation(og[:, c, b_ * 512:(b_ + 1) * 512], bk[:, :], AF.Sigmoid), r=[rbk], w=[rog[c]])
            dump("og", og[:, :, :], rog, "b")
            if stop == "B2":
                return fin_tile()
            dump("qaT", SL[:, SL_QA:SL_QA + 8, :], rsl[SL_QA:SL_QA + 8], "b")
            dump("kaT", SL[:, SL_KA:SL_KA + 2, :], rsl[SL_KA:SL_KA + 2], "b")
            dump("wtok", wtok[:, :], [rwtok], "f")
            if stop == "B4":
                return fin_tile()

            bank_mode["split"] = True
            for c in range(4):
                mixer_chunk(t, c)
            bank_mode["split"] = False
            dump("catT", SL[:, SL_CAT:SL_CAT + 16, :], rsl[SL_CAT:SL_CAT + 16], "b")
            if stop == "C":
                return fin_tile()

            tok_gemm(list(range(SL_CAT, SL_CAT + 16)), 2, [8, 8])
            dump("a", abuf[:, :, :], rog + rvx, "f")
            norm_boundary(0, g_xpre)
            dump("h1", h[:, :, :], rh, "f")
            if stop == "D":
                return fin_tile()

            QX, OX = SL_QK, SL_QK + 8
            for b_ in range(2):
                wb, rw = getw()
                for j in range(4):
                    oc = b_ * 4 + j
                    bk, rbk = nb()
                    for k in range(8):
                        pe(lambda: PE.matmul(bk[:, :], wb[:, k, j * 128:(j + 1) * 128], SL[:, SL_U + k, :], start=(k == 0), stop=(k == 7)),
                           r=[rw, rsl[SL_U + k]], w=[rbk])
                    act(lambda: A.copy(SL[:, QX + oc, :], bk[:, :]), r=[rbk], w=[rsl[QX + oc]])
            for hd in range(4):
                for mc in range(2):
                    bk, rbk = nb()
                    for dc in range(2):
                        pe(lambda: PE.matmul(bk[:, :], KmT[:, hd * 2 + dc, mc * 128:(mc + 1) * 128], SL[:, QX + hd * 2 + dc, :], start=(dc == 0), stop=(dc == 1)),
                           r=[rKm, rsl[QX + hd * 2 + dc]], w=[rbk])
                    act(lambda: A.activation(SL[:, SL_PT + hd * 2 + mc, :], bk[:, :], AF.Exp, scale=1.0 / 16.0), r=[rbk], w=[rsl[SL_PT + hd * 2 + mc]])
            for hd in range(4):
                bkD, rbkD = nb()
                for mc in range(2):
                    pe(lambda: PE.matmul(bkD[:, :], ones_bf, SL[:, SL_PT + hd * 2 + mc, :], start=(mc == 0), stop=(mc == 1)),
                       r=[rcst, rsl[SL_PT + hd * 2 + mc]], w=[rbkD])
                ri = ring("rd", 2)
                act(lambda: A.activation(rd[ri][:], bkD[:, :], AF.Ln), r=[rbkD], w=[rrd[ri]])
                act(lambda: A.activation(rd[ri][:], rd[ri][:], AF.Exp, scale=-1.0), r=[rrd[ri]], w=[rrd[ri]])
                for ec in range(2):
                    bkO, rbkO = nb()
                    for mc in range(2):
                        pe(lambda: PE.matmul(bkO[:, :], Vm[:, mc, hd * 256 + ec * 128:hd * 256 + (ec + 1) * 128], SL[:, SL_PT + hd * 2 + mc, :], start=(mc == 0), stop=(mc == 1)),
                           r=[rVm, rsl[SL_PT + hd * 2 + mc]], w=[rbkO])
                    dve(lambda: V.tensor_tensor(SL[:, OX + hd * 2 + ec, :], bkO[:, :], rd[ri][:], ALU.mult), r=[rbkO, rrd[ri]], w=[rsl[OX + hd * 2 + ec]])
            tok_gemm(list(range(OX, OX + 8)), 1, [8])
            dump("c", abuf[:, :, :], rog + rvx, "f")
            norm_boundary(1, g_fpre)
            dump("h2", h[:, :, :], rh, "f")
            if stop == "E":
                return fin_tile()

            HT = SL_QK
            for i in range(6):
                nj = 4 if i < 5 else 2
                wb, rw = getw()
                for j in range(nj):
                    bk, rbk = nb()
                    for k in range(8):
                        pe(lambda: PE.matmul(bk[:, :], wb[:, k, j * 128:(j + 1) * 128], SL[:, SL_U + k, :], start=(k == 0), stop=(k == 7)),
                           r=[rw, rsl[SL_U + k]], w=[rbk])
                    act(lambda: A.activation(SL[:, HT + i * 4 + j, :], bk[:, :], AF.Silu), r=[rbk], w=[rsl[HT + i * 4 + j]])
                wb, rw = getw()
                for j in range(nj):
                    bk, rbk = nb()
                    for k in range(8):
                        pe(lambda: PE.matmul(bk[:, :], wb[:, k, j * 128:(j + 1) * 128], SL[:, SL_U + k, :], start=(k == 0), stop=(k == 7)),
                           r=[rw, rsl[SL_U + k]], w=[rbk])
                    dve(lambda: V.tensor_tensor(SL[:, HT + i * 4 + j, :], bk[:, :], SL[:, HT + i * 4 + j, :], ALU.mult),
                        r=[rbk, rsl[HT + i * 4 + j]], w=[rsl[HT + i * 4 + j]])
            tok_gemm(list(range(HT, HT + 22)), 3, [8, 8, 6])
            dump("f", abuf[:, :, :], rog + rvx, "f")
            def store_chunk(c):
                out_accs.extend(S.dma("sp", [(y_d[s, t0 + c * 128:t0 + (c + 1) * 128, :], h[:, c, :])], reads=[rh[c]], writes=[ry]))
            norm_boundary(2, None, on_done=store_chunk)

        def gates(bkI, rbkI, bkF, rbkF, first_tile):
            X1, X2, X3 = GX[0], GX[1], GX[2]
            r1, r2, r3 = rgx
            if first_tile:
                dve(lambda: V.memset(mall[0:4, :], 0.0), w=[rmall])
            else:
                dve(lambda: V.tensor_copy(mall[0:4, 0:1], mall[0:4, 4:5]), r=[rmall], w=[rmall])
            act(lambda: A.activation(X1[0:4, :], bkF[0:4, :], AF.Exp, scale=-1.0, bias=gbt[0:4, 2:3]), r=[rbkF, rgbt], w=[r1])
            act(lambda: A.activation(X1[0:4, :], X1[0:4, :], AF.Ln, bias=1.0), r=[r1], w=[r1])
            for c in range(4):
                dve(lambda: V.tensor_tensor_scan(X3[0:4, c * 128:(c + 1) * 128], cf[0:4, CF_ONES4:CF_ONES4 + 128], X1[0:4, c * 128:(c + 1) * 128], 0.0, ALU.mult, ALU.add),
                    r=[r1, rcst], w=[r3])
            dve(lambda: V.scalar_tensor_tensor(X2[0:4, :], bkI[0:4, :], gbt[0:4, 0:1], X3[0:4, :], ALU.add, ALU.add), r=[rbkI, rgbt, r3], w=[r2])
            dve(lambda: V.tensor_reduce(gsm[0:4, 0:4], X2[0:4, :].rearrange("p (a b) -> p a b", b=128), AX.X, ALU.max), r=[r2], w=[rgsm])
            dve(lambda: V.tensor_scalar(gsm[0:4, 4:8], X3[0:4, :].rearrange("p (a b) -> p a b", b=128)[:, :, 127], -1.0, None, ALU.mult), r=[r3], w=[rgsm])
            dve(lambda: V.tensor_tensor_scan(mall[0:4, 1:5], gsm[0:4, 0:4], gsm[0:4, 4:8], mall[0:4, 0:1], ALU.max, ALU.add), r=[rgsm, rmall], w=[rmall])
            dve(lambda: V.tensor_tensor(gsm[0:4, 8:12], mall[0:4, 0:4], gsm[0:4, 0:4], ALU.max), r=[rgsm, rmall], w=[rgsm])
            dve(lambda: V.tensor_scalar(gsm[0:4, 12:16], gsm[0:4, 8:12], -1.0, None, ALU.mult), r=[rgsm], w=[rgsm])
            dve(lambda: V.tensor_scalar(gsm[0:4, 16:20], gsm[0:4, 8:12], -1.0, -LN16, ALU.mult, ALU.add), r=[rgsm], w=[rgsm])
            dve(lambda: V.tensor_tensor(gsm[0:4, 20:24], mall[0:4, 0:4], gsm[0:4, 8:12], ALU.subtract), r=[rgsm, rmall], w=[rgsm])
            act(lambda: A.activation(gsm[0:4, 20:24], gsm[0:4, 20:24], AF.Exp), r=[rgsm], w=[rgsm])
            for c in range(4):
                act(lambda: A.activation(X1[0:4, c * 128:(c + 1) * 128], X2[0:4, c * 128:(c + 1) * 128], AF.Exp, bias=gsm[0:4, 16 + c:17 + c]), r=[r2, rgsm, r1], w=[r1])
                act(lambda: A.activation(X3[0:4, c * 128:(c + 1) * 128], X3[0:4, c * 128:(c + 1) * 128], AF.Exp, bias=gsm[0:4, 12 + c:13 + c]), r=[r3, rgsm], w=[r3])
            bk, rbk = nb()
            for c in range(4):
                pe(lambda: PE.transpose(bk[:, c * 4:(c + 1) * 4], X1[0:4, c * 128:(c + 1) * 128], identf[0:4, 0:4]), r=[r1, rcst], w=[rbk])
                pe(lambda: PE.transpose(bk[:, 16 + c * 4:16 + (c + 1) * 4], X3[0:4, c * 128:(c + 1) * 128], identf[0:4, 0:4]), r=[r3, rcst], w=[rbk])
            dve(lambda: V.tensor_tensor(Rm[0:4, :].rearrange("p (a b) -> p a b", b=4),
                                        gsm[0:4, 20:24].unsqueeze(2).broadcast_to([4, 4, 4]),
                                        cf[0:4, CF_EYE4:CF_EYE4 + 4].unsqueeze(1).broadcast_to([4, 4, 4]), ALU.mult),
                r=[rgsm, rcst], w=[rRm])
            pe(lambda: PE.matmul(bk[:, 32:48], cf[0:4, CF_ONES4:CF_ONES4 + 128], Rm[0:4, :], start=True, stop=True), r=[rRm, rcst], w=[rbk])
            dve(lambda: V.tensor_copy(wtok[:, 0:48], bk[:, 0:48]), r=[rbk], w=[rwtok])

        def mlstm_phases(c):
            cs = slice(c * 128, (c + 1) * 128)
            Q0, K0 = SL_QK, SL_QK + 8
            st_ = {}

            def M1():
                bkS, rbkS = nb()
                bkT, rbkT = nb()
                bvT = bfv(bkT)
                for hd in range(4):
                    for dc in range(2):
                        pe(lambda: PE.matmul(bkS[:, hd * 128:(hd + 1) * 128], SL[:, K0 + hd * 2 + dc, cs], SL[:, Q0 + hd * 2 + dc, cs], start=(dc == 0), stop=(dc == 1)),
                           r=[rsl[K0 + hd * 2 + dc], rsl[Q0 + hd * 2 + dc]], w=[rbkS])
                for i in range(8):
                    pe(lambda: PE.transpose(bvT[:, i, :], SL[:, K0 + i, cs], ident), r=[rsl[K0 + i], rcst], w=[rbkT])
                for hd in range(4):
                    wcol = wtok[:, c * 4 + hd:c * 4 + hd + 1]
                    dcol = wtok[:, 32 + c * 4 + hd:32 + c * 4 + hd + 1]
                    dve(lambda: V.scalar_tensor_tensor(PT[:, hd, :], bkS[:, hd * 128:(hd + 1) * 128], wcol, maskT, ALU.mult, ALU.mult),
                        r=[rbkS, rwtok, rcst], w=[rPT[hd]])
                    act(lambda: A.activation(kw[:, hd, :].rearrange("p (a b) -> p a b", b=128), bvT[:, 2 * hd:2 * hd + 2, :], AF.Copy, scale=wcol),
                        r=[rbkT, rwtok], w=[rkw[hd]])
                    dve(lambda: V.tensor_scalar(Cb[:, hd, :, :], Cst[:, hd, :, :], dcol, None, ALU.mult), r=[rC[hd], rwtok], w=[rCb[hd]])

            def M2():
                for hd in range(4):
                    dcol = wtok[:, 32 + c * 4 + hd:32 + c * 4 + hd + 1]
                    bkN, rbkN = nb()
                    pe(lambda: PE.matmul(bkN[:, 0:257], PT[:, hd, :], vx[:, c, hd, :], start=True, stop=False), r=[rPT[hd], rvx[c]], w=[rbkN])
                    for dc in range(2):
                        pe(lambda: PE.matmul(bkN[:, 0:257], SL[:, Q0 + hd * 2 + dc, cs], Cb[:, hd, dc, :], start=False, stop=(dc == 1)),
                           r=[rsl[Q0 + hd * 2 + dc], rCb[hd]], w=[rbkN])
                    act(lambda: A.copy(nd[:, hd, :], bkN[:, 0:257]), r=[rbkN], w=[rnd[hd]])
                    ji = ring("junk", 1)
                    act(lambda: A.activation(junk[ji][:, 0:256], bkN[:, 0:256], AF.Square, accum_out=fin[:, hd:hd + 1]), r=[rbkN], w=[rjunk[ji], rfin])
                    for dc in range(2):
                        bkU, rbkU = nb()
                        pe(lambda: PE.matmul(bkU[:, 0:257], kw[:, hd, dc * 128:(dc + 1) * 128], vx[:, c, hd, :], start=True, stop=True),
                           r=[rkw[hd], rvx[c]], w=[rbkU])
                        dve(lambda: V.scalar_tensor_tensor(Cst[:, hd, dc, :], Cst[:, hd, dc, :], dcol, bkU[:, 0:257], ALU.mult, ALU.add),
                            r=[rC[hd], rwtok, rbkU, rCb[hd]], w=[rC[hd]])

            def M3():
                den = nd[:, :, 256]
                thr = wtok[:, 16 + c * 4:16 + c * 4 + 4]
                dve(lambda: V.scalar_tensor_tensor(fin[:, 4:8], den, -1.0, den, ALU.mult, ALU.max), r=rnd, w=[rfin])
                dve(lambda: V.tensor_tensor(fin[:, 4:8], fin[:, 4:8], thr, ALU.max), r=[rfin, rwtok], w=[rfin])
                dve(lambda: V.reciprocal(fin[:, 4:8], fin[:, 4:8]), r=[rfin], w=[rfin])
                dve(lambda: V.tensor_tensor(fin[:, 8:12], fin[:, 4:8], fin[:, 4:8], ALU.mult), r=[rfin], w=[rfin])
                dve(lambda: V.tensor_tensor(fin[:, 8:12], fin[:, 8:12], fin[:, 0:4], ALU.mult), r=[rfin], w=[rfin])
                act(lambda: A.activation(fin[:, 8:12], fin[:, 8:12], AF.Ln, scale=1.0 / 256.0, bias=cf[:, CF_EPS:CF_EPS + 1]), r=[rfin, rcst], w=[rfin])
                act(lambda: A.activation(fin[:, 8:12], fin[:, 8:12], AF.Exp, scale=-0.5), r=[rfin], w=[rfin])
                dve(lambda: V.tensor_tensor(fin[:, 12:16], fin[:, 4:8], fin[:, 8:12], ALU.mult), r=[rfin], w=[rfin])
                hi = ring("hmn", 1)
                st_["hi"] = hi
                for hd in range(4):
                    dve(lambda: V.scalar_tensor_tensor(hmn[hi][:, hd * 256:(hd + 1) * 256], nd[:, hd, 0:256], fin[:, 12 + hd:13 + hd], og[:, c, hd * 256:(hd + 1) * 256], ALU.mult, ALU.mult),
                        r=[rnd[hd], rfin, rog[c]], w=[rhmn[hi]])

            def M4():
                hi = st_["hi"]
                bk2, rbk2 = nb()
                bv2 = bfv(bk2)
                for i in range(8):
                    pe(lambda: PE.transpose(bv2[:, i, :], hmn[hi][:, i * 128:(i + 1) * 128], ident), r=[rhmn[hi], rcst], w=[rbk2])
                dve(lambda: V.tensor_tensor(SL[:, SL_CAT:SL_CAT + 8, cs], bv2[:, :, :], g_mls.unsqueeze(2).broadcast_to([128, 8, 128]), ALU.mult),
                    r=[rbk2, rcolp], w=rsl[SL_CAT:SL_CAT + 8])

            return M1, M2, M3, M4

        swa_prev = {"vpad": None}

        def swa_phases(t, c):
            cs = slice(c * 128, (c + 1) * 128)
            has_prev = not (t == 0 and c == 0)
            kinds = [(par, pr) for par in range(2) for pr in ((0, 1) if has_prev else (0,))]
            st_ = {}

            def S0():
                vi = ring("vpad", 3)
                vp, rvp = vpad[vi], rvpad[vi]
                vsrc = vtok[:, c, :].rearrange("p (g d) -> p g d", d=64)
                S.op("pool", lambda: nc.gpsimd.tensor_copy(vp[:, :, 0, 0:64], vsrc), [rvtok[c]], [rvp])
                S.op("pool", lambda: nc.gpsimd.tensor_copy(vp[:, :, 1, 64:128], vsrc), [rvtok[c]], [rvp])
                st_["vp"] = (vp, rvp)
                st_["pv"] = swa_prev["vpad"]
                swa_prev["vpad"] = (vp, rvp)

            def S1(g):
                bks = {}
                for (par, pr) in kinds:
                    bk, rbk = nb("swa")
                    bks[(par, pr)] = (bk, rbk)
                    pe(lambda: PE.matmul(bk[:, :], ident, mprev4 if pr else mcur4, start=True, stop=False), r=[rcst], w=[rbk])
                for pair in range(4):
                    qs = SL_QA + g * 4 + pair
                    for (par, pr) in kinds:
                        bk, rbk = bks[(par, pr)]
                        ps_ = slice(par * 64, (par + 1) * 64)
                        if not pr:
                            kap, kres = SL[ps_, SL_KA + g, cs], rsl[SL_KA + g]
                        elif c > 0:
                            kap, kres = SL[ps_, SL_KA + g, (c - 1) * 128:c * 128], rsl[SL_KA + g]
                        else:
                            kap, kres = prevK[ps_, g, :], rprevK
                        pe(lambda: PE.matmul(bk[:, pair * 128:(pair + 1) * 128], kap, SL[ps_, qs, cs], start=False, stop=(pair == 3)),
                           r=[kres, rsl[qs]], w=[rbk])
                pts = {}
                for n_, (par, pr) in enumerate(kinds):
                    bk, rbk = bks[(par, pr)]
                    sl = SL_PT + g * 4 + n_
                    pts[(par, pr)] = sl
                    act(lambda: A.activation(SL[:, sl, :], bk[:, :], AF.Exp, scale=0.125), r=[rbk], w=[rsl[sl]])
                st_[("pts", g)] = pts

            def S2(g):
                pts = st_[("pts", g)]
                vp, rvp = st_["vp"]
                pv = st_["pv"]
                bkD, rbkD = nb()
                bkO, rbkO = nb()
                pe(lambda: PE.matmul(bkD[:, :], A40[0:40, g, :], cb[0:40, CB_BSEL:CB_BSEL + 512], start=True, stop=False), r=[rAs, rcst], w=[rbkD])
                for n_, (par, pr) in enumerate(kinds):
                    last = (n_ == len(kinds) - 1)
                    sl = pts[(par, pr)]
                    pe(lambda: PE.matmul(bkD[:, :], onesB if par else onesA, SL[:, sl, :], start=False, stop=last), r=[rcst, rsl[sl]], w=[rbkD])
                    vsrc_t, vres = (pv if pr else (vp, rvp))
                    pe(lambda: PE.matmul(bkO[:, :], vsrc_t[:, g, par, :], SL[:, sl, :], start=(n_ == 0), stop=last), r=[vres, rsl[sl]], w=[rbkO])
                ri = ring("rd", 2)
                act(lambda: A.activation(rd[ri][:], bkD[:, :], AF.Ln), r=[rbkD], w=[rrd[ri]])
                act(lambda: A.activation(rd[ri][:], rd[ri][:], AF.Exp, scale=-1.0), r=[rrd[ri]], w=[rrd[ri]])
                dve(lambda: V.tensor_tensor(SL[:, SL_CAT + 8 + g * 4:SL_CAT + 8 + g * 4 + 4, cs],
                                            bkO[:, :].rearrange("p (a b) -> p a b", b=128),
                                            rd[ri][:].rearrange("p (a b) -> p a b", b=128), ALU.mult),
                    r=[rbkO, rrd[ri]], w=rsl[SL_CAT + 8 + g * 4:SL_CAT + 8 + g * 4 + 4])

            return S0, S1, S2

        def mixer_chunk(t, c):
            M1, M2, M3, M4 = mlstm_phases(c)
            S0, S1, S2 = swa_phases(t, c)
            if stop == "C-nomlstm":
                S0(); S1(0); S1(1); S2(0); S2(1)
                return
            if stop == "C-noswa":
                M1(); M2(); M3(); M4()
                return
            S0()
            M1()
            S1(0)
            M2()
            S1(1)
            S2(0)
            M3()
            S2(1)
            M4()

        for s in range(nseq):
            if not nomem:
                mem_stage(s)
            for hd in range(4):
                dve(lambda: V.memset(Cst[:, hd, :, :], 0.0), w=[rC[hd]])
            for t in range(ntile):
                tile(s, t)
        for a_ in out_accs:
            S._wait("sp", a_)
        S.wait_all("sp", rwb)
        stats = dict(inst=S.n_inst, waits=S.n_wait, incs=S.n_inc)
    return nc, dbg_items, stats


_CONSTS = None


def make_in_maps(inputs):
    global _CONSTS
    if _CONSTS is None:
        _CONSTS = _consts()
    cbv, cfv = _CONSTS
    f = lambda a: np.ascontiguousarray(np.asarray(a, dtype=np.float32))
    conv = f(inputs["conv_qk"][0])
    colp = np.concatenate([
        conv.reshape(4 * 16, 128),
        f(inputs["mix_pre_g"][0]).reshape(8, 128),
        f(inputs["xattn_pre_g"][0]).reshape(8, 128),
        f(inputs["ffn_pre_g"][0]).reshape(8, 128),
        f(inputs["mem_norm_g"][0]).reshape(8, 128),
        f(inputs["mlstm_norm_g"][0]).reshape(8, 128),
        np.zeros((24, 128), np.float32),
    ], axis=0)
    rowg = np.stack([f(inputs["mix_post_g"][0]), f(inputs["xattn_post_g"][0]), f(inputs["ffn_post_g"][0])], axis=0)
    gb = np.stack([f(inputs["i_bias"][0]), f(inputs["f_bias"][0])], axis=1)
    sk = np.ascontiguousarray(f(inputs["attn_sinks"][0]).reshape(2, 8).T)
    shared = dict(
        w_in=f(inputs["w_in"][0]), w_out=f(inputs["w_out"][0]), w_xq=f(inputs["w_xq"][0]),
        w_xkv=f(inputs["w_xkv"][0]), w_xo=f(inputs["w_xo"][0]), w_gu=f(inputs["w_gate_up"][0]),
        w_dn=f(inputs["w_down"][0]), colp=np.ascontiguousarray(colp), rowg=np.ascontiguousarray(rowg),
        gb=np.ascontiguousarray(gb), sk=sk, cstb=cbv, cstf=cfv)
    x = f(inputs["x"])
    mem = f(inputs["mem"])
    pos = np.ascontiguousarray(np.asarray(inputs["positions"], dtype=np.int32))
    maps = []
    for i in range(NCORES):
        m = dict(shared)
        m["x"] = x[2 * i:2 * i + 2]
        m["mem"] = mem[2 * i:2 * i + 2]
        m["pos"] = pos[2 * i:2 * i + 2]
        maps.append(m)
    return maps


def kernel(**inputs):
    nc, _, _ = build()
    maps = make_in_maps(inputs)
    res = run_bass_kernel_spmd(nc, maps, core_ids=list(range(NCORES)))
    out = np.concatenate([np.asarray(r["y"]) for r in res.results], axis=0)
    return out.astype(np.float32)
```
